# Optimizing a Trainium2 kernel written in Bass

```python
import jax, jax.numpy as jnp
from jax import lax
import numpy as np


D_MODEL = 4096
BATCH = 2
SEQ = 8192
DEPTH = 1

CHUNK = 64
HEAD_DIM = 128
N_HEADS_ATT = 16
N_HEADS_DN = 16
ATT_WIDTH = N_HEADS_ATT * HEAD_DIM
DN_WIDTH = N_HEADS_DN * HEAD_DIM
MIX_WIDTH = ATT_WIDTH + DN_WIDTH
LEFT_CHUNKS = 8
BAND = (LEFT_CHUNKS + 1) * CHUNK
REL_CLIP = 256
CONV_K = 4
N_GROUPS = 8
EXPERTS_PER_GROUP = 8
N_EXPERTS = N_GROUPS * EXPERTS_PER_GROUP
TOP_K_IN_GROUP = 2
D_EXPERT = 512
MOE_BLOCK = 128
EPS = 1e-6

OFF_ATT = 0
OFF_DN_QKV = OFF_ATT + 3 * ATT_WIDTH
OFF_DN_GATE = OFF_DN_QKV + 3 * DN_WIDTH
OFF_DN_BETA = OFF_DN_GATE + DN_WIDTH
OFF_DN_DECAY = OFF_DN_BETA + N_HEADS_DN
PROJ_COLS = OFF_DN_DECAY + N_HEADS_DN

kernel_name = 'hybrid_band_attn_gdn_hmoe'


def rms_norm(x, w):
    xf = x.astype(jnp.float32)
    y = xf * lax.rsqrt(jnp.mean(xf * xf, axis=-1, keepdims=True) + EPS)
    return (y * w.astype(jnp.float32)).astype(x.dtype)


def l2_norm(x):
    return x * lax.rsqrt(jnp.sum(x * x, axis=-1, keepdims=True) + EPS)


def causal_short_conv(x, w):
    s = x.shape[1]
    xp = jnp.pad(x, ((0, 0), (CONV_K - 1, 0), (0, 0)))
    y = xp[:, 0:s] * w[0]
    for i in range(1, CONV_K):
        y = y + xp[:, i:i + s] * w[i]
    return jax.nn.silu(y)


def band_attention(q, k, v, rel_bias):
    bsz, nh, s, dh = q.shape
    nc = s // CHUNK
    qc = jnp.moveaxis(q.reshape(bsz, nh, nc, CHUNK, dh), 2, 0)
    pad = ((0, 0), (0, 0), (LEFT_CHUNKS * CHUNK, 0), (0, 0))
    kp = jnp.pad(k, pad)
    vp = jnp.pad(v, pad)
    qi = jnp.arange(CHUNK)[:, None]
    kj = jnp.arange(BAND)
    rel = (LEFT_CHUNKS * CHUNK + qi) - kj[None, :]
    bias = rel_bias[:, jnp.clip(rel, -REL_CLIP, REL_CLIP) + REL_CLIP].astype(jnp.float32)
    scale = HEAD_DIM ** -0.5

    def one_chunk(inp):
        c, q_blk = inp
        k_blk = lax.dynamic_slice_in_dim(kp, c * CHUNK, BAND, axis=2)
        v_blk = lax.dynamic_slice_in_dim(vp, c * CHUNK, BAND, axis=2)
        sc = jnp.einsum('bhqd,bhkd->bhqk', q_blk, k_blk).astype(jnp.float32) * scale + bias
        valid = (c * CHUNK - LEFT_CHUNKS * CHUNK + kj) >= 0
        sc = jnp.where(valid, sc, -jnp.inf)
        pr = jax.nn.softmax(sc, axis=-1)
        return jnp.einsum('bhqk,bhkd->bhqd', pr.astype(v.dtype), v_blk)

    out = lax.map(one_chunk, (jnp.arange(nc), qc))
    return jnp.moveaxis(out, 0, 2).reshape(bsz, nh, s, dh)


def gated_delta_rule(q, k, v, g, beta):
    bsz, nh, s, dk = q.shape
    dv = v.shape[-1]
    nc = s // CHUNK
    q = q.reshape(bsz, nh, nc, CHUNK, dk)
    k = k.reshape(bsz, nh, nc, CHUNK, dk)
    v = v.reshape(bsz, nh, nc, CHUNK, dv)
    g = g.reshape(bsz, nh, nc, CHUNK)
    beta = beta.reshape(bsz, nh, nc, CHUNK)
    gc = jnp.cumsum(g, axis=-1)
    idx = jnp.arange(CHUNK)
    incl = idx[:, None] >= idx[None, :]
    strict = idx[:, None] > idx[None, :]
    gamma = jnp.exp(jnp.where(incl, gc[..., :, None] - gc[..., None, :], -jnp.inf))
    kbeta = k * beta[..., None]
    m = jnp.where(strict, jnp.einsum('bhnid,bhnjd->bhnij', kbeta, k) * gamma, 0.0)
    a = m + jnp.eye(CHUNK, dtype=m.dtype)
    rhs = jnp.concatenate([v * beta[..., None], kbeta * jnp.exp(gc)[..., None]], axis=-1)
    sol = lax.linalg.triangular_solve(a, rhs, left_side=True, lower=True, unit_diagonal=True)
    u = sol[..., :dv]
    w = sol[..., dv:]
    att = jnp.einsum('bhnid,bhnjd->bhnij', q, k) * gamma

    def step(state, inp):
        q_c, k_c, u_c, w_c, att_c, gc_c = inp
        v_new = u_c - jnp.einsum('bhck,bhkv->bhcv', w_c, state)
        o_c = (jnp.einsum('bhck,bhkv->bhcv', q_c * jnp.exp(gc_c)[..., None], state)
               + jnp.einsum('bhij,bhjv->bhiv', att_c, v_new))
        g_last = gc_c[..., -1]
        k_dec = k_c * jnp.exp(g_last[..., None] - gc_c)[..., None]
        state = state * jnp.exp(g_last)[..., None, None] + jnp.einsum('bhck,bhcv->bhkv', k_dec, v_new)
        return state, o_c

    xs = tuple(jnp.moveaxis(t, 2, 0) for t in (q, k, u, w, att, gc))
    state0 = jnp.zeros((bsz, nh, dk, dv), jnp.float32)
    _, o = lax.scan(step, state0, xs)
    return jnp.moveaxis(o, 0, 2).reshape(bsz, nh, s, dv)


def hier_moe(h, w_group, w_router, w_gate, w_up, w_down):
    bsz, s, d = h.shape
    t = bsz * s
    hf = h.reshape(t, d)
    grp_prob = jax.nn.softmax((hf @ w_group).astype(jnp.float32), axis=-1)
    grp_w, grp_idx = lax.top_k(grp_prob, 1)
    exp_logits = (hf @ w_router).astype(jnp.float32).reshape(t, N_GROUPS, EXPERTS_PER_GROUP)
    in_grp = jnp.take_along_axis(exp_logits, grp_idx[:, :, None], axis=1)[:, 0]
    top_w, top_i = lax.top_k(jax.nn.softmax(in_grp, axis=-1), TOP_K_IN_GROUP)
    top_w = top_w / jnp.sum(top_w, axis=-1, keepdims=True)
    gate = grp_w * top_w
    expert = grp_idx * EXPERTS_PER_GROUP + top_i

    n_assign = t * TOP_K_IN_GROUP
    flat_e = expert.reshape(n_assign)
    flat_tok = jnp.repeat(jnp.arange(t, dtype=jnp.int32), TOP_K_IN_GROUP)
    flat_g = gate.reshape(n_assign)
    order = jnp.argsort(flat_e)
    e_s = flat_e[order]
    tok_s = flat_tok[order]
    g_s = flat_g[order]
    counts = jax.ops.segment_sum(jnp.ones_like(flat_e), flat_e, num_segments=N_EXPERTS)
    padded = (counts + MOE_BLOCK - 1) // MOE_BLOCK * MOE_BLOCK
    pad_end = jnp.cumsum(padded)
    pad_start = pad_end - padded
    start = jnp.cumsum(counts) - counts
    dest = pad_start[e_s] + (jnp.arange(n_assign, dtype=jnp.int32) - start[e_s])
    n_slots = n_assign + N_EXPERTS * MOE_BLOCK
    n_blocks = n_slots // MOE_BLOCK
    buf_tok = jnp.zeros((n_slots,), jnp.int32).at[dest].set(tok_s)
    blk_e = jnp.minimum(jnp.searchsorted(pad_end, jnp.arange(n_blocks, dtype=jnp.int32) * MOE_BLOCK, side='right'), N_EXPERTS - 1)

    def expert_block(inp):
        tok, e = inp
        xb = hf[tok]
        hid = jax.nn.silu(xb @ w_gate[e]) * (xb @ w_up[e])
        return hid @ w_down[e]

    ys = lax.map(expert_block, (buf_tok.reshape(n_blocks, MOE_BLOCK), blk_e)).reshape(n_slots, d)
    out = jnp.zeros((t, d), h.dtype).at[tok_s].add(ys[dest] * g_s[:, None].astype(h.dtype))
    return out.reshape(bsz, s, d)


def hybrid_layer(x, norm1_w, w_in, q_norm_w, k_norm_w, rel_bias, conv_w, a_log, dt_bias,
                 o_norm_w, w_out, norm2_w, w_group, w_router, w_gate, w_up, w_down):
    bsz, s, _ = x.shape
    h = rms_norm(x, norm1_w)
    p = h @ w_in

    def heads(tn, nh):
        return tn.reshape(bsz, s, nh, HEAD_DIM).transpose(0, 2, 1, 3)

    qa = rms_norm(heads(p[..., OFF_ATT:OFF_ATT + ATT_WIDTH], N_HEADS_ATT), q_norm_w)
    ka = rms_norm(heads(p[..., OFF_ATT + ATT_WIDTH:OFF_ATT + 2 * ATT_WIDTH], N_HEADS_ATT), k_norm_w)
    va = heads(p[..., OFF_ATT + 2 * ATT_WIDTH:OFF_DN_QKV], N_HEADS_ATT)
    ya = band_attention(qa, ka, va, rel_bias).transpose(0, 2, 1, 3).reshape(bsz, s, ATT_WIDTH)

    qkv_b = causal_short_conv(p[..., OFF_DN_QKV:OFF_DN_GATE], conv_w).astype(jnp.float32)
    qb = l2_norm(heads(qkv_b[..., 0:DN_WIDTH], N_HEADS_DN)) * (HEAD_DIM ** -0.5)
    kb = l2_norm(heads(qkv_b[..., DN_WIDTH:2 * DN_WIDTH], N_HEADS_DN))
    vb = heads(qkv_b[..., 2 * DN_WIDTH:3 * DN_WIDTH], N_HEADS_DN)
    z = p[..., OFF_DN_GATE:OFF_DN_BETA].astype(jnp.float32).reshape(bsz, s, N_HEADS_DN, HEAD_DIM)
    beta = jax.nn.sigmoid(p[..., OFF_DN_BETA:OFF_DN_DECAY].astype(jnp.float32)).transpose(0, 2, 1)
    g = (-jnp.exp(a_log.astype(jnp.float32))
         * jax.nn.softplus(p[..., OFF_DN_DECAY:PROJ_COLS].astype(jnp.float32) + dt_bias.astype(jnp.float32))).transpose(0, 2, 1)
    ob = gated_delta_rule(qb, kb, vb, g, beta).transpose(0, 2, 1, 3)
    ob = rms_norm(ob, o_norm_w) * jax.nn.silu(z)
    yb = ob.reshape(bsz, s, DN_WIDTH).astype(x.dtype)

    x = x + jnp.concatenate([ya, yb], axis=-1) @ w_out
    x = x + hier_moe(rms_norm(x, norm2_w), w_group, w_router, w_gate, w_up, w_down)
    return x


def setup_inputs(seed: int = 0) -> dict:
    key = jax.random.key(seed)
    ks = jax.random.split(key, 20)
    f32 = jnp.float32
    nrm = lambda k, shape, sc: jax.random.normal(k, shape, f32) * sc
    dt = jnp.exp(jax.random.uniform(ks[8], (DEPTH, N_HEADS_DN), f32, np.log(1e-3), np.log(1e-1)))
    return {
        'x': jax.random.normal(ks[0], (BATCH, SEQ, D_MODEL), f32),
        'norm1_w': 1.0 + nrm(ks[1], (DEPTH, D_MODEL), 0.02),
        'w_in': nrm(ks[2], (DEPTH, D_MODEL, PROJ_COLS), D_MODEL ** -0.5),
        'q_norm_w': 1.0 + nrm(ks[3], (DEPTH, HEAD_DIM), 0.02),
        'k_norm_w': 1.0 + nrm(ks[4], (DEPTH, HEAD_DIM), 0.02),
        'rel_bias': nrm(ks[5], (DEPTH, N_HEADS_ATT, 2 * REL_CLIP + 1), 0.5),
        'conv_w': nrm(ks[6], (DEPTH, CONV_K, 3 * DN_WIDTH), CONV_K ** -0.5),
        'a_log': jnp.log(jax.random.uniform(ks[7], (DEPTH, N_HEADS_DN), f32, 1.0, 16.0)),
        'dt_bias': dt + jnp.log(-jnp.expm1(-dt)),
        'o_norm_w': 1.0 + nrm(ks[9], (DEPTH, HEAD_DIM), 0.02),
        'w_out': nrm(ks[10], (DEPTH, MIX_WIDTH, D_MODEL), MIX_WIDTH ** -0.5),
        'norm2_w': 1.0 + nrm(ks[11], (DEPTH, D_MODEL), 0.02),
        'w_group': nrm(ks[12], (DEPTH, D_MODEL, N_GROUPS), D_MODEL ** -0.5),
        'w_router': nrm(ks[13], (DEPTH, D_MODEL, N_EXPERTS), D_MODEL ** -0.5),
        'w_gate': nrm(ks[14], (DEPTH, N_EXPERTS, D_MODEL, D_EXPERT), D_MODEL ** -0.5),
        'w_up': nrm(ks[15], (DEPTH, N_EXPERTS, D_MODEL, D_EXPERT), D_MODEL ** -0.5),
        'w_down': nrm(ks[16], (DEPTH, N_EXPERTS, D_EXPERT, D_MODEL), D_EXPERT ** -0.5),
    }


def reference(x, norm1_w, w_in, q_norm_w, k_norm_w, rel_bias, conv_w, a_log, dt_bias,
              o_norm_w, w_out, norm2_w, w_group, w_router, w_gate, w_up, w_down):
    for l in range(DEPTH):
        x = hybrid_layer(x, norm1_w[l], w_in[l], q_norm_w[l], k_norm_w[l], rel_bias[l], conv_w[l],
                         a_log[l], dt_bias[l], o_norm_w[l], w_out[l], norm2_w[l], w_group[l],
                         w_router[l], w_gate[l], w_up[l], w_down[l])
    return x
```

```python
import contextlib
import numpy as np
import concourse.bass as bass
import concourse.mybir as mybir

F32 = mybir.dt.float32
BF16 = mybir.dt.bfloat16
I32 = mybir.dt.int32
ALU = mybir.AluOpType
AF = mybir.ActivationFunctionType
AX = mybir.AxisListType

ENGS = ("pe", "act", "dve", "pool", "sp")


class Buf:
    _n = 0

    def __init__(self, name=None):
        Buf._n += 1
        self.name = name or f"b{Buf._n}"
        self.last_write = None
        self.readers = []
        self.dsem = f"d_{self.name}_{Buf._n}"
        self.dcnt = 0
        self.excl = False


class Prog:
    def __init__(self, nc, same_engine_sync=True):
        self.nc = nc
        self.q = {e: [] for e in ENGS}
        self.cnt = {e: 0 for e in ENGS}
        self.waited = {e: {} for e in ENGS}
        self.semkeys = {}
        self.same = same_engine_sync

    def _deps(self, reads, writes):
        deps = []
        for b in reads:
            if b.last_write is not None:
                deps.append(b.last_write)
        for b in writes:
            if b.last_write is not None:
                deps.append(b.last_write)
            deps.extend(b.readers)
        return deps

    def _waits(self, eng, deps):
        waits = []
        best = {}
        for (sk, val, src) in deps:
            if src == eng and (eng == "pe" or not self.same):
                continue
            if self.waited[eng].get(sk, 0) >= val:
                continue
            if best.get(sk, 0) < val:
                best[sk] = val
        for sk, val in best.items():
            self.waited[eng][sk] = val
            waits.append((sk, val))
        return waits

    def op(self, eng, fn, reads=(), writes=()):
        writes = list(writes) + [b for b in reads if b.excl]
        reads = [b for b in reads if not b.excl]
        waits = self._waits(eng, self._deps(reads, writes))
        self.cnt[eng] += 1
        sk = "e_" + eng
        self.semkeys[sk] = None
        tok = (sk, self.cnt[eng], eng)
        self.q[eng].append((waits, fn, (sk, 1)))
        for b in reads:
            b.readers.append(tok)
        for b in writes:
            b.last_write = tok
            b.readers = []
        return tok

    def dma(self, eng, fn, sbuf, reads=(), writes=()):
        deps = self._deps(reads, writes)
        if sbuf.dcnt > 0:
            deps.append((sbuf.dsem, 16 * sbuf.dcnt, "dma"))
        waits = self._waits(eng, deps)
        sbuf.dcnt += 1
        self.semkeys[sbuf.dsem] = None
        tok = (sbuf.dsem, 16 * sbuf.dcnt, "dma")
        self.q[eng].append((waits, fn, (sbuf.dsem, 16)))
        for b in reads:
            b.readers.append(tok)
        for b in writes:
            b.last_write = tok
            b.readers = []
        return tok

    def final_wait(self, eng, bufs):
        deps = []
        for b in bufs:
            if b.last_write is not None:
                deps.append(b.last_write)
            deps.extend(b.readers)
        waits = self._waits(eng, deps)
        self.q[eng].append((waits, None, None))

    def emit(self):
        nc = self.nc
        with contextlib.ExitStack() as st:
            sems = {}
            for sk in self.semkeys:
                sems[sk] = st.enter_context(nc.semaphore(sk))
            block = st.enter_context(nc.Block())
            engmap = {"pe": block.tensor, "act": block.scalar, "dve": block.vector,
                      "pool": block.gpsimd, "sp": block.sync}

            def mk(eng):
                def body(e):
                    for waits, fn, inc in self.q[eng]:
                        for sk, val in waits:
                            e.wait_ge(sems[sk], val)
                        if fn is not None:
                            ins = fn(e)
                            ins.then_inc(sems[inc[0]], inc[1])
                return body

            for eng in ENGS:
                if self.q[eng]:
                    engmap[eng](mk(eng))
        print("PROG: insts", {e: len(self.q[e]) for e in ENGS}, "sems", len(self.semkeys))

from concourse.bass_utils import run_bass_kernel_spmd

NCORES = 8


def new_nc():
    return bass.Bass("TRN2", target_bir_lowering=False)


def dram_in(nc, name, shape, dt=F32):
    return nc.dram_tensor(name, list(shape), dt, kind="ExternalInput").ap()


def dram_out(nc, name, shape, dt=F32):
    return nc.dram_tensor(name, list(shape), dt, kind="ExternalOutput").ap()


def build_proj(T, D, N, norm, has_res, eps=1e-6, TG=1024):
    nc = new_nc()
    KT = D // 128
    TG = min(TG, T)
    aT = dram_in(nc, "aT", [D, T])
    W = dram_in(nc, "W", [D, N])
    if norm:
        a = dram_in(nc, "a", [T, D])
        cs = dram_in(nc, "cs", [128, KT])
    if has_res:
        res = dram_in(nc, "res", [T, N])
    out = dram_out(nc, "out", [T, N])
    P = Prog(nc)
    A = nc.alloc_sbuf_tensor
    hT = A("hT", [128, KT, TG], BF16); b_hT = [Buf("hT%d" % i) for i in range(KT)]
    stg = [A("stg%d" % i, [128, TG], F32) for i in range(2)]; b_stg = [Buf("stg%d" % i) for i in range(2)]
    NQ = 4 if KT % 4 == 0 else 1
    KQ = KT // NQ
    wb = [A("wb%d" % i, [128, KT, 512], BF16) for i in range(2)]
    b_wb = [[Buf("wb%d_%d" % (i, q)) for q in range(NQ)] for i in range(2)]
    ost = [A("ost%d" % i, [128, 512], F32) for i in range(3)]; b_ost = [Buf("ost%d" % i) for i in range(3)]
    ps = [nc.alloc_psum_tensor("ps%d" % i, [128, 512], F32) for i in range(2)]; b_ps = [Buf("ps%d" % i) for i in range(2)]
    for b in b_ps:
        b.excl = True
    nsub_all = T // 128
    rstd = A("rstd", [128, nsub_all], F32); b_rstd = Buf("rstd")
    if norm:
        csb = A("csb", [128, KT], F32); b_cs = Buf("cs")
        P.dma("sp", lambda e: e.dma_start(out=csb[:], in_=cs), b_cs, writes=[b_cs])
        at = [A("at%d" % i, [128, D], F32) for i in range(2)]; b_at = [Buf("at%d" % i) for i in range(2)]
        junk = A("junk", [128, D], BF16); b_junk = Buf("junk")
        for s in range(nsub_all):
            i = s % 2
            P.dma("sp", (lambda i, s: lambda e: e.dma_start(out=at[i][:], in_=a[s * 128:(s + 1) * 128, :]))(i, s), b_at[i], writes=[b_at[i]])
            P.op("act", (lambda i, s: lambda e: e.activation(junk[:], at[i][:], AF.Square, accum_out=rstd[:, s:s + 1]))(i, s),
                 reads=[b_at[i]], writes=[b_junk, b_rstd])
        P.op("act", lambda e: e.activation(rstd[:], rstd[:], AF.Sqrt, bias=eps, scale=1.0 / D), reads=[b_rstd], writes=[b_rstd])
        P.op("dve", lambda e: e.reciprocal(rstd[:], rstd[:]), reads=[b_rstd], writes=[b_rstd])
    nchunks = (N + 511) // 512
    wcount = 0
    ocount = 0
    pcount = 0
    for g in range(T // TG):
        t0 = g * TG
        for kt in range(KT):
            i = kt % 2
            P.dma("sp", (lambda i, kt, t0: lambda e: e.dma_start(out=stg[i][:], in_=aT[kt * 128:(kt + 1) * 128, t0:t0 + TG]))(i, kt, t0),
                  b_stg[i], writes=[b_stg[i]])
            if norm:
                P.op("act", (lambda i, kt: lambda e: e.activation(hT[:, kt, :], stg[i][:], AF.Identity, scale=csb[:, kt:kt + 1]))(i, kt),
                     reads=[b_stg[i], b_cs], writes=[b_hT[kt]])
            else:
                P.op("act", (lambda i, kt: lambda e: e.copy(hT[:, kt, :], stg[i][:]))(i, kt), reads=[b_stg[i]], writes=[b_hT[kt]])
        for n in range(nchunks):
            c0 = n * 512
            cw = min(512, N - c0)
            wi = wcount % 2
            wcount += 1
            Wr = W.rearrange("(kt p) n -> p kt n", p=128)
            for q in range(NQ):
                P.dma("pool", (lambda wi, q, c0, cw: lambda e: e.dma_start(out=wb[wi][:, q * KQ:(q + 1) * KQ, 0:cw],
                                                                            in_=Wr[:, q * KQ:(q + 1) * KQ, c0:c0 + cw]))(wi, q, c0, cw),
                      b_wb[wi][q], writes=[b_wb[wi][q]])
            for s in range(TG // 128):
                sg = g * (TG // 128) + s
                pi = pcount % 2
                pcount += 1
                for kt in range(KT):
                    P.op("pe", (lambda pi, kt, s, wi, cw: lambda e: e.matmul(ps[pi][:, 0:cw], hT[:, kt, s * 128:(s + 1) * 128], wb[wi][:, kt, 0:cw],
                                                                               start=(kt == 0), stop=(kt == KT - 1)))(pi, kt, s, wi, cw),
                         reads=[b_hT[kt], b_wb[wi][kt // KQ]], writes=[b_ps[pi]])
                oi = ocount % 3
                ocount += 1
                r0 = t0 + s * 128
                if norm:
                    P.op("act", (lambda oi, pi, cw, sg: lambda e: e.activation(ost[oi][:, 0:cw], ps[pi][:, 0:cw], AF.Identity, scale=rstd[:, sg:sg + 1]))(oi, pi, cw, sg),
                         reads=[b_ps[pi], b_rstd], writes=[b_ost[oi]])
                elif has_res:
                    P.dma("sp", (lambda oi, r0, c0, cw: lambda e: e.dma_start(out=ost[oi][:, 0:cw], in_=res[r0:r0 + 128, c0:c0 + cw]))(oi, r0, c0, cw),
                          b_ost[oi], writes=[b_ost[oi]])
                    P.op("dve", (lambda oi, pi, cw: lambda e: e.tensor_tensor(ost[oi][:, 0:cw], ps[pi][:, 0:cw], ost[oi][:, 0:cw], op=ALU.add))(oi, pi, cw),
                         reads=[b_ps[pi], b_ost[oi]], writes=[b_ost[oi]])
                else:
                    P.op("act", (lambda oi, pi, cw: lambda e: e.copy(ost[oi][:, 0:cw], ps[pi][:, 0:cw]))(oi, pi, cw), reads=[b_ps[pi]], writes=[b_ost[oi]])
                P.dma("sp", (lambda oi, r0, c0, cw: lambda e: e.dma_start(out=out[r0:r0 + 128, c0:c0 + cw], in_=ost[oi][:, 0:cw]))(oi, r0, c0, cw),
                      b_ost[oi], reads=[b_ost[oi]])
    P.final_wait("sp", b_ost)
    P.emit()
    return nc


class K:
    def __init__(self, nc):
        self.nc = nc
        self.P = Prog(nc)

    def o(self, eng, method, *args, r=(), w=(), **kw):
        return self.P.op(eng, lambda e: getattr(e, method)(*args, **kw), reads=r, writes=w)

    def d(self, eng, out, in_, sb, r=(), w=()):
        return self.P.dma(eng, lambda e: e.dma_start(out=out, in_=in_), sb, reads=r, writes=w)

    def sb(self, name, shape, dt=F32):
        return self.nc.alloc_sbuf_tensor(name, list(shape), dt), Buf(name)

    def ps(self, name, shape=None, dt=F32):
        b = Buf(name)
        b.excl = True
        return self.nc.alloc_psum_tensor(name, [128, 512], F32), b

    def consts(self):
        ones, b1 = self.sb("c_ones", [128, 128]); ident, b2 = self.sb("c_ident", [128, 128])
        ui, b3 = self.sb("c_ui", [64, 64]); us, b4 = self.sb("c_us", [64, 64])
        self.o("pool", "memset", ones[:], 1.0, w=[b1])
        self.o("pool", "memset", ident[:], 1.0, w=[b2])
        self.o("pool", "affine_select", out=ident[:], in_=ident[:], pattern=[[-1, 128]], compare_op=ALU.is_equal, fill=0.0,
               base=0, channel_multiplier=1, r=[b2], w=[b2])
        self.o("pool", "memset", ui[:], 1.0, w=[b3])
        self.o("pool", "memset", us[:], 1.0, w=[b4])
        self.o("pool", "affine_select", out=ui[:], in_=ui[:], pattern=[[1, 64]], compare_op=ALU.is_ge, fill=0.0,
               base=0, channel_multiplier=-1, r=[b3], w=[b3])
        self.o("pool", "affine_select", out=us[:], in_=us[:], pattern=[[1, 64]], compare_op=ALU.is_gt, fill=0.0,
               base=0, channel_multiplier=-1, r=[b4], w=[b4])
        return (ones, b1), (ident, b2), (ui, b3), (us, b4)

    def finish(self, eng, bufs):
        self.P.final_wait(eng, bufs)
        self.P.emit()
        return self.nc


def build_attn(NP, S, eps=1e-6):
    nc = new_nc()
    k = K(nc)
    qT = dram_in(nc, "qT", [NP, 128, S]); kT = dram_in(nc, "kT", [NP, 128, S]); v = dram_in(nc, "v", [NP, S, 128])
    qw = dram_in(nc, "qw", [128, 1]); kw = dram_in(nc, "kw", [128, 1]); biasT = dram_in(nc, "biasT", [NP, 5, 128, 128])
    ya = dram_out(nc, "ya", [NP, S, 128])
    (ones, b_ones), _, _, _ = k.consts()
    NB = S // 128
    PW = min(512, S)
    raw, b_raw = k.sb("raw", [128, S]);
    qn, b_qn = k.sb("qn", [128, S], BF16); kn, b_kn = k.sb("kn", [128, S], BF16)
    sq, b_sq = k.sb("sq", [128, PW]); rr, b_rr = k.sb("rr", [128, PW])
    v1, b_v1 = k.sb("v1", [128, NB, 132], BF16)
    bT, b_bT = k.sb("bT", [128, 5, 128])
    wq, b_wq = k.sb("wq", [128, 1]); wk, b_wk = k.sb("wk", [128, 1])
    tS = [k.sb("tS%d" % i, [128, 128]) for i in range(2)]
    E = [k.sb("E%d" % i, [128, 128], BF16) for i in range(2)]
    rc, b_rc = k.sb("rc", [128, 1])
    ost = [k.sb("aost%d" % i, [128, 128]) for i in range(2)]
    psN, b_psN = k.ps("psN", [128, PW])
    psS = [k.ps("psS%d" % i, [128, 128]) for i in range(2)]
    psO = [k.ps("psO%d" % i, [128, 132]) for i in range(2)]
    k.d("sp", wq[:], qw, b_wq, w=[b_wq]); k.d("sp", wk[:], kw, b_wk, w=[b_wk])
    k.o("pool", "memset", v1[:], 1.0, w=[b_v1])
    scale = 128.0 ** -0.5
    cnt = 0
    for p in range(NP):
        for (src, dst, b_dst, wt, b_wt) in ((qT, qn, b_qn, wq, b_wq), (kT, kn, b_kn, wk, b_wk)):
            k.d("sp", raw[:], src[p], b_raw, w=[b_raw])
            for c0 in range(0, S, PW):
                k.o("act", "activation", sq[:], raw[:, c0:c0 + PW], AF.Square, r=[b_raw], w=[b_sq])
                k.o("pe", "matmul", psN[:], ones[:], sq[:], start=True, stop=True, r=[b_ones, b_sq], w=[b_psN])
                k.o("act", "activation", rr[:], psN[:], AF.Sqrt, bias=eps, scale=1.0 / 128, r=[b_psN], w=[b_rr])
                k.o("dve", "reciprocal", rr[:], rr[:], r=[b_rr], w=[b_rr])
                k.o("dve", "scalar_tensor_tensor", dst[:, c0:c0 + PW], raw[:, c0:c0 + PW], wt[:, 0:1], rr[:], op0=ALU.mult, op1=ALU.mult,
                    r=[b_raw, b_wt, b_rr], w=[b_dst])
        vr = v[p].rearrange("(n p) d -> p n d", p=128)
        for n0 in range(0, NB, 8):
            n1 = min(NB, n0 + 8)
            k.d("pool", v1[:, n0:n1, 0:128], vr[:, n0:n1, :], b_v1, w=[b_v1])
        k.d("sp", bT[:], biasT[p].rearrange("j k q -> k j q"), b_bT, w=[b_bT])
        for qb in range(NB):
            kts = [kt for kt in range(qb - 4, qb + 1) if kt >= 0]
            po, b_po = psO[qb % 2]
            for idx, kt in enumerate(kts):
                j = kt - (qb - 4)
                i = cnt % 2
                cnt += 1
                pS, b_pS = psS[i]; t_, b_t = tS[i]; e_, b_e = E[i]
                k.o("pe", "matmul", pS[:, 0:128], kn[:, kt * 128:(kt + 1) * 128], qn[:, qb * 128:(qb + 1) * 128], start=True, stop=True,
                    r=[b_kn, b_qn], w=[b_pS])
                k.o("dve", "scalar_tensor_tensor", t_[:], pS[:, 0:128], scale, bT[:, j, :], op0=ALU.mult, op1=ALU.add, r=[b_pS, b_bT], w=[b_t])
                k.o("act", "activation", e_[:], t_[:], AF.Exp, r=[b_t], w=[b_e])
                k.o("pe", "matmul", po[:, 0:129], e_[:], v1[:, kt, 0:129], start=(idx == 0), stop=(idx == len(kts) - 1),
                    r=[b_e, b_v1], w=[b_po])
            os_, b_os = ost[qb % 2]
            k.o("dve", "reciprocal", rc[:], po[:, 128:129], r=[b_po], w=[b_rc])
            k.o("act", "activation", os_[:], po[:, 0:128], AF.Identity, scale=rc[:, 0:1], r=[b_po, b_rc], w=[b_os])
            k.d("sp", ya[p, qb * 128:(qb + 1) * 128, :], os_[:], b_os, r=[b_os])
    return k.finish("sp", [b for (_, b) in ost])


def build_dnpre(NP, S, eps=1e-6):
    nc = new_nc()
    k = K(nc)
    pT = dram_in(nc, "pT", [NP, 3, 128, S]); cw = dram_in(nc, "cw", [NP, 3, 128, 4])
    pb = dram_in(nc, "pb", [NP, 128, S // 128]); pg = dram_in(nc, "pg", [NP, 128, S // 128])
    alog = dram_in(nc, "alog", [NP, 128, 1]); dtb = dram_in(nc, "dtb", [NP, 128, 1])
    oT = dram_out(nc, "oT", [NP, 3, 128, S]); obeta = dram_out(nc, "obeta", [NP, 128, S // 128]); og = dram_out(nc, "og", [NP, 128, S // 128])
    (ones, b_ones), _, _, _ = k.consts()
    PW = min(512, S)
    xin = [k.sb("xin%d" % i, [128, S + 3]) for i in range(2)]
    y = [k.sb("y%d" % i, [128, S]) for i in range(2)]
    cwt, b_cwt = k.sb("cwt", [128, 4])
    sq, b_sq = k.sb("sq", [128, PW]); rr, b_rr = k.sb("rr", [128, PW])
    psN, b_psN = k.ps("psN", [128, PW])
    sm = [k.sb("sm%d" % i, [128, S // 128]) for i in range(4)]
    al, b_al = k.sb("al", [128, 1]); db, b_db = k.sb("db", [128, 1])
    cnt = 0
    for i in range(2):
        k.o("pool", "memset", xin[i][0][:, 0:3], 0.0, w=[xin[i][1]])
    for p in range(NP):
        for c in range(3):
            xi, b_xi = xin[cnt % 2]; yy, b_y = y[cnt % 2]
            cnt += 1
            k.d("sp", xi[:, 3:S + 3], pT[p, c], b_xi, w=[b_xi])
            k.d("sp", cwt[:], cw[p, c], b_cwt, w=[b_cwt])
            k.o("dve", "tensor_scalar", yy[:], xi[:, 0:S], cwt[:, 0:1], None, op0=ALU.mult, r=[b_xi, b_cwt], w=[b_y])
            for t in range(1, 4):
                k.o("dve", "scalar_tensor_tensor", yy[:], xi[:, t:S + t], cwt[:, t:t + 1], yy[:], op0=ALU.mult, op1=ALU.add,
                    r=[b_xi, b_cwt, b_y], w=[b_y])
            k.o("act", "activation", yy[:], yy[:], AF.Silu, r=[b_y], w=[b_y])
            if c < 2:
                sc = (128.0 ** -0.5) if c == 0 else 1.0
                for c0 in range(0, S, PW):
                    k.o("act", "activation", sq[:], yy[:, c0:c0 + PW], AF.Square, r=[b_y], w=[b_sq])
                    k.o("pe", "matmul", psN[:], ones[:], sq[:], start=True, stop=True, r=[b_ones, b_sq], w=[b_psN])
                    k.o("act", "activation", rr[:], psN[:], AF.Sqrt, bias=eps, scale=1.0, r=[b_psN], w=[b_rr])
                    k.o("dve", "reciprocal", rr[:], rr[:], r=[b_rr], w=[b_rr])
                    k.o("dve", "scalar_tensor_tensor", yy[:, c0:c0 + PW], yy[:, c0:c0 + PW], sc, rr[:], op0=ALU.mult, op1=ALU.mult,
                        r=[b_y, b_rr], w=[b_y])
            k.d("sp", oT[p, c], yy[:], b_y, r=[b_y])
        (s0, b0), (s1, b1), (s2, b2), (s3, b3) = sm
        k.d("sp", s0[:], pb[p], b0, w=[b0]); k.d("sp", s1[:], pg[p], b1, w=[b1])
        k.d("sp", al[:], alog[p], b_al, w=[b_al]); k.d("sp", db[:], dtb[p], b_db, w=[b_db])
        k.o("act", "activation", s2[:], s0[:], AF.Sigmoid, r=[b0], w=[b2])
        k.d("sp", obeta[p], s2[:], b2, r=[b2])
        k.o("act", "activation", s3[:], s1[:], AF.Exp, bias=db[:, 0:1], r=[b1, b_db], w=[b3])
        k.o("act", "activation", s3[:], s3[:], AF.Ln, bias=1.0, r=[b3], w=[b3])
        k.o("act", "activation", al[:], al[:], AF.Exp, r=[b_al], w=[b_al])
        k.o("dve", "tensor_scalar", s3[:], s3[:], al[:, 0:1], -1.0, op0=ALU.mult, op1=ALU.mult, r=[b3, b_al], w=[b3])
        k.d("sp", og[p], s3[:], b3, r=[b3])
    return k.finish("sp", [b for (_, b) in y] + [b for (_, b) in sm])


def build_dn(NP, S, GC=16):
    nc = new_nc()
    k = K(nc)
    NCH = S // 64
    GC = min(GC, NCH)
    qT = dram_in(nc, "qT", [NP, 128, S]); kT = dram_in(nc, "kT", [NP, 128, S])
    kc = dram_in(nc, "kc", [NP, 64, NCH, 128]); vc = dram_in(nc, "vc", [NP, 64, NCH, 128])
    beta = dram_in(nc, "beta", [NP, 64, NCH]); g = dram_in(nc, "g", [NP, 64, NCH])
    o = dram_out(nc, "o", [NP, S, 128])
    (ones, b_ones), (ident, b_id), (ui, b_ui), (us, b_us) = k.consts()
    qTs, b_qT = k.sb("qTs", [128, GC * 64]); kTs, b_kT = k.sb("kTs", [128, GC * 64])
    kcs, b_kc = k.sb("kcs", [64, GC, 128]); vcs, b_vc = k.sb("vcs", [64, GC, 128])
    bts, b_bt = k.sb("bts", [64, NCH]); gs, b_g = k.sb("gs", [64, NCH])
    Gb, b_Gb = k.sb("Gb", [64, 64]); Bb, b_Bb = k.sb("Bb", [64, 64])
    gcs, b_gcs = k.sb("gcs", [64, 1]); gl, b_gl = k.sb("gl", [128, 1])
    egc, b_egc = k.sb("egc", [64, 1]); egl, b_egl = k.sb("egl", [128, 1]); dec, b_dec = k.sb("dec", [64, 1]); be, b_be = k.sb("be", [64, 1])
    tS, b_tS = k.sb("tS", [64, 64]); ET, b_ET = k.sb("ET", [64, 64]); EU, b_EU = k.sb("EU", [64, 64]); EB, b_EB = k.sb("EB", [64, 64])
    attT, b_attT = k.sb("attT", [64, 64])
    Pm = [k.sb("Pm%d" % i, [64, 64]) for i in range(2)]; PT = [k.sb("PTm%d" % i, [64, 64]) for i in range(2)]
    TT, b_TT = k.sb("TT", [64, 64])
    vb, b_vb = k.sb("vb", [64, 128]); kb, b_kb = k.sb("kb", [64, 128]); kd, b_kd = k.sb("kd", [64, 128])
    us_, b_us_ = k.sb("u_s", [64, 128]); wT, b_wT = k.sb("wT", [128, 64]); vn, b_vn = k.sb("vn", [64, 128])
    o2s, b_o2s = k.sb("o2s", [64, 128])
    ost, b_ost = k.sb("dost", [64, GC, 128])
    St, b_S = k.sb("St", [128, 128])
    psA, b_psA = k.ps("psA", [128, 512]); psB, b_psB = k.ps("psB", [128, 512]); psC, b_psC = k.ps("psC", [128, 512])
    psD, b_psD = k.ps("psD", [128, 512]); psE, b_psE = k.ps("psE", [128, 512]); psU, b_psU = k.ps("psU", [128, 512])
    psV, b_psV = k.ps("psV", [128, 512]); psS, b_psS = k.ps("psS", [128, 512])
    b_psW = b_psU; b_psO1 = b_psV; b_psO2 = b_psV
    MM = dict(start=True, stop=True)
    for p in range(NP):
        k.d("sp", bts[:], beta[p], b_bt, w=[b_bt]); k.d("sp", gs[:], g[p], b_g, w=[b_g])
        k.o("dve", "memset", St[:], 0.0, w=[b_S])
        for n in range(NCH):
            c = n % GC
            if c == 0:
                t0 = n * 64
                k.d("sp", qTs[:], qT[p, :, t0:t0 + GC * 64], b_qT, w=[b_qT]); k.d("sp", kTs[:], kT[p, :, t0:t0 + GC * 64], b_kT, w=[b_kT])
                k.d("sp", kcs[:], kc[p, :, n:n + GC, :], b_kc, w=[b_kc]); k.d("sp", vcs[:], vc[p, :, n:n + GC, :], b_vc, w=[b_vc])
            qTc = qTs[:, c * 64:(c + 1) * 64]; kTc = kTs[:, c * 64:(c + 1) * 64]
            gcol = gs[:, n:n + 1]; bcol = bts[:, n:n + 1]
            k.o("dve", "tensor_scalar", Gb[:], ones[0:64, 0:64], gcol, None, op0=ALU.mult, r=[b_ones, b_g], w=[b_Gb])
            k.o("dve", "tensor_scalar", Bb[:], ones[0:64, 0:64], bcol, None, op0=ALU.mult, r=[b_ones, b_bt], w=[b_Bb])
            k.o("pe", "matmul", psA[0:64, 0:64], Gb[:], ui[:], r=[b_Gb, b_ui], w=[b_psA], **MM)
            k.o("pe", "matmul", psA[0:64, 64:65], ui[:], gcol, r=[b_ui, b_g], w=[b_psA], **MM)
            k.o("pe", "matmul", psA[0:64, 128:192], Bb[:], ident[0:64, 0:64], r=[b_Bb, b_id], w=[b_psA], **MM)
            k.o("pe", "matmul", psA[0:128, 192:193], ones[0:64, 0:128], gcol, r=[b_ones, b_g], w=[b_psA], **MM)
            k.o("dve", "tensor_copy", gcs[:], psA[0:64, 64:65], r=[b_psA], w=[b_gcs])
            k.o("dve", "tensor_copy", gl[:], psA[0:128, 192:193], r=[b_psA], w=[b_gl])
            k.o("act", "activation", egc[:], gcs[:], AF.Exp, r=[b_gcs], w=[b_egc])
            k.o("act", "activation", egl[:], gl[:], AF.Exp, r=[b_gl], w=[b_egl])
            k.o("act", "activation", dec[:], gcs[:], AF.Exp, bias=gl[0:64, 0:1], scale=-1.0, r=[b_gcs, b_gl], w=[b_dec])
            k.o("dve", "tensor_tensor", be[:], bcol, egc[:], op=ALU.mult, r=[b_bt, b_egc], w=[b_be])
            k.o("dve", "tensor_scalar", tS[:], psA[0:64, 0:64], gcs[:, 0:1], 0.0, op0=ALU.subtract, op1=ALU.min, r=[b_psA, b_gcs], w=[b_tS])
            k.o("act", "activation", ET[:], tS[:], AF.Exp, r=[b_tS], w=[b_ET])
            k.o("dve", "tensor_tensor", EU[:], ET[:], ui[:], op=ALU.mult, r=[b_ET, b_ui], w=[b_EU])
            k.o("dve", "tensor_tensor", EB[:], psA[0:64, 128:192], ET[:], op=ALU.mult, r=[b_psA, b_ET], w=[b_EB])
            k.o("dve", "tensor_tensor", EB[:], EB[:], us[:], op=ALU.mult, r=[b_EB, b_us], w=[b_EB])
            k.o("pe", "matmul", psB[0:64, 0:64], kTc, kTc, r=[b_kT], w=[b_psB], **MM)
            k.o("pe", "matmul", psB[0:64, 64:128], kTc, qTc, r=[b_kT, b_qT], w=[b_psB], **MM)
            k.o("dve", "tensor_tensor", attT[:], psB[0:64, 64:128], EU[:], op=ALU.mult, r=[b_psB, b_EU], w=[b_attT])
            (P0, b_P0), (P1, b_P1) = Pm; (T0, b_T0), (T1, b_T1) = PT
            k.o("dve", "scalar_tensor_tensor", T0[:], psB[0:64, 0:64], -1.0, EB[:], op0=ALU.mult, op1=ALU.mult, r=[b_psB, b_EB], w=[b_T0])
            k.o("pe", "transpose", psC[0:64, 0:64], T0[:], ident[0:64, 0:64], r=[b_T0, b_id], w=[b_psC])
            k.o("act", "copy", P0[:], psC[0:64, 0:64], r=[b_psC], w=[b_P0])
            k.o("dve", "tensor_tensor", TT[:], T0[:], ident[0:64, 0:64], op=ALU.add, r=[b_T0, b_id], w=[b_TT])
            cur = 0
            for lv in range(5):
                Pc, b_Pc = Pm[cur]; Tc, b_Tc = PT[cur]; Pn, b_Pn = Pm[1 - cur]; Tn, b_Tn = PT[1 - cur]
                k.o("pe", "matmul", psC[0:64, 64:128], Tc[:], Pc[:], r=[b_Tc, b_Pc], w=[b_psC], **MM)
                if lv < 4:
                    k.o("pe", "matmul", psD[0:64, 0:64], Pc[:], Tc[:], r=[b_Tc, b_Pc], w=[b_psD], **MM)
                k.o("act", "copy", Pn[:], psC[0:64, 64:128], r=[b_psC], w=[b_Pn])
                if lv < 4:
                    k.o("dve", "tensor_copy", Tn[:], psD[0:64, 0:64], r=[b_psD], w=[b_Tn])
                k.o("pe", "matmul", psE[0:64, 0:64], Pn[:], TT[:], r=[b_Pn, b_TT], w=[b_psE], **MM)
                k.o("dve", "tensor_tensor", TT[:], TT[:], psE[0:64, 0:64], op=ALU.add, r=[b_TT, b_psE], w=[b_TT])
                cur = 1 - cur
            k.o("dve", "tensor_scalar", vb[:], vcs[:, c, :], bcol, None, op0=ALU.mult, r=[b_vc, b_bt], w=[b_vb])
            k.o("dve", "tensor_scalar", kb[:], kcs[:, c, :], be[:, 0:1], None, op0=ALU.mult, r=[b_kc, b_be], w=[b_kb])
            k.o("act", "activation", kd[:], kcs[:, c, :], AF.Identity, scale=dec[:, 0:1], r=[b_kc, b_dec], w=[b_kd])
            k.o("pe", "matmul", psU[0:64, 0:128], TT[:], vb[:], r=[b_TT, b_vb], w=[b_psU], **MM)
            k.o("pe", "matmul", psU[0:128, 128:192], kb[:], TT[:], r=[b_TT, b_kb], w=[b_psW], **MM)
            k.o("act", "copy", us_[:], psU[0:64, 0:128], r=[b_psU], w=[b_us_])
            k.o("dve", "tensor_copy", wT[:], psU[0:128, 128:192], r=[b_psW], w=[b_wT])
            k.o("pe", "matmul", psV[0:64, 0:128], wT[:], St[:], r=[b_wT, b_S], w=[b_psV], **MM)
            k.o("pe", "matmul", psV[0:64, 128:256], qTc, St[:], r=[b_qT, b_S], w=[b_psO1], **MM)
            k.o("dve", "tensor_tensor", vn[:], us_[:], psV[0:64, 0:128], op=ALU.subtract, r=[b_us_, b_psV], w=[b_vn])
            k.o("pe", "matmul", psV[0:64, 256:384], attT[:], vn[:], r=[b_attT, b_vn], w=[b_psO2], **MM)
            k.o("pe", "matmul", psS[0:128, 0:128], kd[:], vn[:], r=[b_kd, b_vn], w=[b_psS], **MM)
            k.o("act", "copy", o2s[:], psV[0:64, 256:384], r=[b_psO2], w=[b_o2s])
            k.o("dve", "scalar_tensor_tensor", ost[:, c, :], psV[0:64, 128:256], egc[:, 0:1], o2s[:], op0=ALU.mult, op1=ALU.add,
                r=[b_psO1, b_egc, b_o2s], w=[b_ost])
            k.o("dve", "scalar_tensor_tensor", St[:], St[:], egl[:, 0:1], psS[0:128, 0:128], op0=ALU.mult, op1=ALU.add,
                r=[b_S, b_egl, b_psS], w=[b_S])
            if c == GC - 1:
                n0 = n - (GC - 1)
                k.d("sp", o[p, n0 * 64:(n + 1) * 64, :].rearrange("(n c) d -> c n d", c=64), ost[:], b_ost, r=[b_ost])
    return k.finish("sp", [b_ost])


def build_ogate(T, H, eps=1e-6):
    nc = new_nc()
    k = K(nc)
    W_ = H * 128
    o = dram_in(nc, "o", [T, W_]); z = dram_in(nc, "z", [T, W_]); onw = dram_in(nc, "onw", [128, 128])
    yb = dram_out(nc, "yb", [T, W_])
    ot = [k.sb("ot%d" % i, [128, W_]) for i in range(2)]; zt = [k.sb("zt%d" % i, [128, W_]) for i in range(2)]
    wt, b_wt = k.sb("wt", [128, 128]); junk, b_junk = k.sb("junk", [128, 128])
    ss, b_ss = k.sb("ss", [128, H])
    k.d("sp", wt[:], onw, b_wt, w=[b_wt])
    for s in range(T // 128):
        (o_, b_o), (z_, b_z) = ot[s % 2], zt[s % 2]
        k.d("sp", o_[:], o[s * 128:(s + 1) * 128, :], b_o, w=[b_o]); k.d("sp", z_[:], z[s * 128:(s + 1) * 128, :], b_z, w=[b_z])
        k.o("act", "activation", z_[:], z_[:], AF.Silu, r=[b_z], w=[b_z])
        for h in range(H):
            k.o("act", "activation", junk[:], o_[:, h * 128:(h + 1) * 128], AF.Square, accum_out=ss[:, h:h + 1], r=[b_o], w=[b_junk, b_ss])
        k.o("act", "activation", ss[:], ss[:], AF.Sqrt, bias=eps, scale=1.0 / 128, r=[b_ss], w=[b_ss])
        k.o("dve", "reciprocal", ss[:], ss[:], r=[b_ss], w=[b_ss])
        for h in range(H):
            sl = slice(h * 128, (h + 1) * 128)
            k.o("dve", "scalar_tensor_tensor", o_[:, sl], o_[:, sl], ss[:, h:h + 1], wt[:], op0=ALU.mult, op1=ALU.mult, r=[b_o, b_ss, b_wt], w=[b_o])
            k.o("dve", "tensor_tensor", o_[:, sl], o_[:, sl], z_[:, sl], op=ALU.mult, r=[b_o, b_z], w=[b_o])
        k.d("sp", yb[s * 128:(s + 1) * 128, :], o_[:], b_o, r=[b_o])
    return k.finish("sp", [b for (_, b) in ot])


def build_router(T, D, eps=1e-6, TG=256):
    nc = new_nc()
    k = K(nc)
    KT = D // 128
    TG = min(TG, T)
    a = dram_in(nc, "a", [T, D]); aT = dram_in(nc, "aT", [D, T]); cs = dram_in(nc, "cs", [128, KT]); csb = dram_in(nc, "csb", [128, D])
    Wr = dram_in(nc, "Wr", [D, 72]); iot = dram_in(nc, "iot", [128, 16])
    h2 = dram_out(nc, "h2", [T, D]); route = dram_out(nc, "route", [T, 4])
    at = [k.sb("at%d" % i, [128, D]) for i in range(2)]
    junk, b_junk = k.sb("junk", [128, D], BF16)
    cst, b_cs = k.sb("cst", [128, KT]); cbt, b_cb = k.sb("cbt", [128, D]); wr, b_wr = k.sb("wr", [128, KT, 72]); io, b_io = k.sb("io", [128, 16])
    stg = [k.sb("stg%d" % i, [128, TG]) for i in range(2)]
    hT, b_hT = k.sb("hT", [128, KT, TG])
    rs, b_rs = k.sb("rs", [128, 1])
    L, b_L = k.sb("L", [128, 72])
    sm, b_sm = k.sb("sm", [128, 16])
    gm, b_gm = k.sb("gm", [128, 8]); ge, b_ge = k.sb("ge", [128, 8]); ig, b_ig = k.sb("ig", [128, 8]); i2, b_i2 = k.sb("i2", [128, 8])
    m1k, b_m1k = k.sb("m1k", [128, 8]); m2k, b_m2k = k.sb("m2k", [128, 8]); tmp, b_tmp = k.sb("tmp", [128, 8])
    rt = [k.sb("rt%d" % i, [128, 4]) for i in range(2)]
    psL, b_psL = k.ps("psL")
    k.d("sp", cst[:], cs, b_cs, w=[b_cs]); k.d("sp", cbt[:], csb, b_cb, w=[b_cb]); k.d("sp", io[:], iot, b_io, w=[b_io])
    Wrr = Wr.rearrange("(kt p) n -> p kt n", p=128)
    for k0 in range(0, KT, 8):
        k1 = min(KT, k0 + 8)
        k.d("sp", wr[:, k0:k1, :], Wrr[:, k0:k1, :], b_wr, w=[b_wr])
    for g in range(T // TG):
        t0 = g * TG
        for kt in range(KT):
            s_, b_s = stg[kt % 2]
            k.d("sp", s_[:], aT[kt * 128:(kt + 1) * 128, t0:t0 + TG], b_s, w=[b_s])
            k.o("act", "activation", hT[:, kt, :], s_[:], AF.Identity, scale=cst[:, kt:kt + 1], r=[b_s, b_cs], w=[b_hT])
        for s in range(TG // 128):
            r0 = t0 + s * 128
            a_, b_a = at[s % 2]
            k.d("sp", a_[:], a[r0:r0 + 128, :], b_a, w=[b_a])
            k.o("act", "activation", junk[:], a_[:], AF.Square, accum_out=rs[:, 0:1], r=[b_a], w=[b_junk, b_rs])
            k.o("act", "activation", rs[:], rs[:], AF.Sqrt, bias=eps, scale=1.0 / D, r=[b_rs], w=[b_rs])
            k.o("dve", "reciprocal", rs[:], rs[:], r=[b_rs], w=[b_rs])
            for kt in range(KT):
                k.o("pe", "matmul", psL[:, 0:72], hT[:, kt, s * 128:(s + 1) * 128], wr[:, kt, :], start=(kt == 0), stop=(kt == KT - 1),
                    r=[b_hT, b_wr], w=[b_psL])
            k.o("act", "activation", L[:], psL[:, 0:72], AF.Identity, scale=rs[:, 0:1], r=[b_psL, b_rs], w=[b_L])
            k.o("dve", "scalar_tensor_tensor", a_[:], a_[:], rs[:, 0:1], cbt[:], op0=ALU.mult, op1=ALU.mult, r=[b_a, b_rs, b_cb], w=[b_a])
            k.d("sp", h2[r0:r0 + 128, :], a_[:], b_a, r=[b_a])
            k.o("dve", "reduce_max", sm[:, 0:1], L[:, 0:8], axis=AX.X, r=[b_L], w=[b_sm])
            k.o("dve", "tensor_scalar", gm[:], L[:, 0:8], sm[:, 0:1], None, op0=ALU.is_equal, r=[b_L, b_sm], w=[b_gm])
            k.o("dve", "tensor_scalar", sm[:, 1:2], sm[:, 0:1], -1.0, None, op0=ALU.mult, r=[b_sm], w=[b_sm])
            k.o("act", "activation", ge[:], L[:, 0:8], AF.Exp, bias=sm[:, 1:2], accum_out=sm[:, 2:3], r=[b_L, b_sm], w=[b_ge, b_sm])
            k.o("dve", "reciprocal", sm[:, 3:4], sm[:, 2:3], r=[b_sm], w=[b_sm])
            k.o("dve", "tensor_scalar", ig[:], L[:, 8:16], gm[:, 0:1], None, op0=ALU.mult, r=[b_L, b_gm], w=[b_ig])
            for gi in range(1, 8):
                k.o("dve", "scalar_tensor_tensor", ig[:], L[:, 8 + gi * 8:16 + gi * 8], gm[:, gi:gi + 1], ig[:], op0=ALU.mult, op1=ALU.add,
                    r=[b_L, b_gm, b_ig], w=[b_ig])
            k.o("dve", "reduce_max", sm[:, 4:5], ig[:], axis=AX.X, r=[b_ig], w=[b_sm])
            k.o("dve", "tensor_scalar", m1k[:], ig[:], sm[:, 4:5], None, op0=ALU.is_equal, r=[b_ig, b_sm], w=[b_m1k])
            k.o("dve", "scalar_tensor_tensor", i2[:], m1k[:], -1e30, ig[:], op0=ALU.mult, op1=ALU.add, r=[b_m1k, b_ig], w=[b_i2])
            k.o("dve", "reduce_max", sm[:, 5:6], i2[:], axis=AX.X, r=[b_i2], w=[b_sm])
            k.o("dve", "tensor_scalar", m2k[:], i2[:], sm[:, 5:6], None, op0=ALU.is_equal, r=[b_i2, b_sm], w=[b_m2k])
            k.o("dve", "tensor_tensor", sm[:, 6:7], sm[:, 4:5], sm[:, 5:6], op=ALU.subtract, r=[b_sm], w=[b_sm])
            k.o("act", "activation", sm[:, 7:8], sm[:, 6:7], AF.Sigmoid, r=[b_sm], w=[b_sm])
            k.o("act", "activation", sm[:, 8:9], sm[:, 6:7], AF.Sigmoid, scale=-1.0, r=[b_sm], w=[b_sm])
            k.o("dve", "tensor_tensor", tmp[:], gm[:], io[:, 0:8], op=ALU.mult, r=[b_gm, b_io], w=[b_tmp])
            k.o("dve", "reduce_sum", sm[:, 9:10], tmp[:], axis=AX.X, r=[b_tmp], w=[b_sm])
            k.o("dve", "tensor_tensor", tmp[:], m1k[:], io[:, 8:16], op=ALU.mult, r=[b_m1k, b_io], w=[b_tmp])
            k.o("dve", "reduce_sum", sm[:, 10:11], tmp[:], axis=AX.X, r=[b_tmp], w=[b_sm])
            k.o("dve", "tensor_tensor", tmp[:], m2k[:], io[:, 8:16], op=ALU.mult, r=[b_m2k, b_io], w=[b_tmp])
            k.o("dve", "reduce_sum", sm[:, 11:12], tmp[:], axis=AX.X, r=[b_tmp], w=[b_sm])
            r_, b_r = rt[s % 2]
            k.o("dve", "tensor_tensor", r_[:, 0:1], sm[:, 9:10], sm[:, 10:11], op=ALU.add, r=[b_sm], w=[b_r])
            k.o("dve", "tensor_tensor", r_[:, 1:2], sm[:, 9:10], sm[:, 11:12], op=ALU.add, r=[b_sm, b_r], w=[b_r])
            k.o("dve", "tensor_tensor", r_[:, 2:3], sm[:, 3:4], sm[:, 7:8], op=ALU.mult, r=[b_sm, b_r], w=[b_r])
            k.o("dve", "tensor_tensor", r_[:, 3:4], sm[:, 3:4], sm[:, 8:9], op=ALU.mult, r=[b_sm, b_r], w=[b_r])
            k.d("sp", route[r0:r0 + 128, :], r_[:], b_r, r=[b_r])
    return k.finish("sp", [b for (_, b) in at] + [b for (_, b) in rt])


def build_experts(D, NE, CAP, DE=512, NS=256):
    nc = new_nc()
    k = K(nc)
    KT = D // 128; HT = DE // 128
    NS = min(NS, CAP)
    XT = dram_in(nc, "XT", [D, NE * CAP]); wg = dram_in(nc, "wg", [NE, D, DE]); wu = dram_in(nc, "wu", [NE, D, DE]); wd = dram_in(nc, "wd", [NE, DE, D])
    Y = dram_out(nc, "Y", [NE * CAP, D])
    NQ = 4 if KT % 4 == 0 else 1
    KQ = KT // NQ
    wgb = [k.nc.alloc_sbuf_tensor("wgb%d" % i, [128, KT, DE], BF16) for i in range(2)]
    wub = [k.nc.alloc_sbuf_tensor("wub%d" % i, [128, KT, DE], BF16) for i in range(2)]
    b_wg = [[Buf("wg%d_%d" % (i, q)) for q in range(NQ)] for i in range(2)]; b_wu = [[Buf("wu%d_%d" % (i, q)) for q in range(NQ)] for i in range(2)]
    wdb = k.nc.alloc_sbuf_tensor("wdb", [128, HT, D], BF16); b_wd = [Buf("wd%d" % h) for h in range(HT)]
    xt = k.nc.alloc_sbuf_tensor("xt", [128, KT, NS], BF16); b_xt = [Buf("xt%d" % q) for q in range(NQ)]
    sg, b_sg = k.sb("sg", [128, NS]); hid, b_hid = k.sb("hid", [128, HT, NS], BF16)
    yst = [k.sb("yst%d" % i, [128, 512]) for i in range(3)]
    psG = [k.ps("psG%d" % i) for i in range(2)]; psU = [k.ps("psU%d" % i) for i in range(2)]; psY = [k.ps("psY%d" % i) for i in range(2)]
    XTr = XT.rearrange("(kt p) s -> p kt s", p=128)
    yc = 0
    for le in range(NE):
        wi = le % 2
        wgr = wg[le].rearrange("(kt p) n -> p kt n", p=128); wur = wu[le].rearrange("(kt p) n -> p kt n", p=128)
        wdr = wd[le].rearrange("(ht p) n -> p ht n", p=128)
        for q in range(NQ):
            k.d("pool", wgb[wi][:, q * KQ:(q + 1) * KQ, :], wgr[:, q * KQ:(q + 1) * KQ, :], b_wg[wi][q], w=[b_wg[wi][q]])
            k.d("pool", wub[wi][:, q * KQ:(q + 1) * KQ, :], wur[:, q * KQ:(q + 1) * KQ, :], b_wu[wi][q], w=[b_wu[wi][q]])
        for h in range(HT):
            k.d("pool", wdb[:, h, :], wdr[:, h, :], b_wd[h], w=[b_wd[h]])
        for sc in range(CAP // NS):
            s0 = le * CAP + sc * NS
            for q in range(NQ):
                k.d("pool", xt[:, q * KQ:(q + 1) * KQ, :], XTr[:, q * KQ:(q + 1) * KQ, s0:s0 + NS], b_xt[q], w=[b_xt[q]])
            for ht in range(HT):
                (pg_, b_pg), (pu_, b_pu) = psG[ht % 2], psU[ht % 2]
                for kt in range(KT):
                    k.o("pe", "matmul", pg_[:, 0:NS], wgb[wi][:, kt, ht * 128:(ht + 1) * 128], xt[:, kt, :], start=(kt == 0), stop=(kt == KT - 1),
                        r=[b_wg[wi][kt // KQ], b_xt[kt // KQ]], w=[b_pg])
                for kt in range(KT):
                    k.o("pe", "matmul", pu_[:, 0:NS], wub[wi][:, kt, ht * 128:(ht + 1) * 128], xt[:, kt, :], start=(kt == 0), stop=(kt == KT - 1),
                        r=[b_wu[wi][kt // KQ], b_xt[kt // KQ]], w=[b_pu])
                k.o("act", "activation", sg[:], pg_[:, 0:NS], AF.Silu, r=[b_pg], w=[b_sg])
                k.o("dve", "tensor_tensor", hid[:, ht, :], sg[:], pu_[:, 0:NS], op=ALU.mult, r=[b_sg, b_pu], w=[b_hid])
            for st in range(NS // 128):
                for cc in range(D // 512):
                    py, b_py = psY[yc % 2]; ys, b_ys = yst[yc % 3]
                    for ht in range(HT):
                        k.o("pe", "matmul", py[:, 0:512], hid[:, ht, st * 128:(st + 1) * 128], wdb[:, ht, cc * 512:(cc + 1) * 512],
                            start=(ht == 0), stop=(ht == HT - 1), r=[b_hid, b_wd[ht]], w=[b_py])
                    if yc % 2 == 0:
                        k.o("act", "copy", ys[:], py[:, 0:512], r=[b_py], w=[b_ys])
                    else:
                        k.o("dve", "tensor_copy", ys[:], py[:, 0:512], r=[b_py], w=[b_ys])
                    k.d("sp", Y[s0 + st * 128:s0 + (st + 1) * 128, cc * 512:(cc + 1) * 512], ys[:], b_ys, r=[b_ys])
                    yc += 1
    return k.finish("sp", [b for (_, b) in yst])


def build_combine(T, D):
    nc = new_nc()
    k = K(nc)
    x1 = dram_in(nc, "x1", [T, D]); Y0 = dram_in(nc, "Y0", [T, D]); Y1 = dram_in(nc, "Y1", [T, D]); gt = dram_in(nc, "gt", [T, 2])
    out = dram_out(nc, "out", [T, D])
    xa = [k.sb("xa%d" % i, [128, D]) for i in range(2)]; ya = [k.sb("ya%d" % i, [128, D]) for i in range(2)]; yb = [k.sb("yb%d" % i, [128, D]) for i in range(2)]
    gg = [k.sb("gg%d" % i, [128, 2]) for i in range(2)]
    for s in range(T // 128):
        i = s % 2
        rows = slice(s * 128, (s + 1) * 128)
        (x_, b_x), (a_, b_a), (b_, b_b), (g_, b_g) = xa[i], ya[i], yb[i], gg[i]
        k.d("sp", x_[:], x1[rows, :], b_x, w=[b_x]); k.d("sp", a_[:], Y0[rows, :], b_a, w=[b_a]); k.d("sp", b_[:], Y1[rows, :], b_b, w=[b_b])
        k.d("sp", g_[:], gt[rows, :], b_g, w=[b_g])
        k.o("dve", "scalar_tensor_tensor", x_[:], a_[:], g_[:, 0:1], x_[:], op0=ALU.mult, op1=ALU.add, r=[b_a, b_g, b_x], w=[b_x])
        k.o("dve", "scalar_tensor_tensor", x_[:], b_[:], g_[:, 1:2], x_[:], op0=ALU.mult, op1=ALU.add, r=[b_b, b_g, b_x], w=[b_x])
        k.d("sp", out[rows, :], x_[:], b_x, r=[b_x])
    return k.finish("sp", [b for (_, b) in xa])


HD = 128
NH = 16
AW = NH * HD
OFF_ATT = 0
OFF_DN_QKV = 3 * AW
OFF_DN_GATE = OFF_DN_QKV + 3 * AW
OFF_DN_BETA = OFF_DN_GATE + AW
OFF_DN_DECAY = OFF_DN_BETA + NH
PROJ_COLS = OFF_DN_DECAY + NH
NEXP = 64


def _c(a):
    return np.ascontiguousarray(a, dtype=np.float32)


def _run(nc, maps):
    res = run_bass_kernel_spmd(nc, maps, core_ids=list(range(len(maps))))
    return res.results


def _feat_layout(vec, D):
    return _c(vec.reshape(D // 128, 128).T)


def _make_biasT(rb):
    kk = np.arange(640)[:, None]; qq = np.arange(128)[None, :]
    idx = np.clip((512 + qq) - kk, -256, 256) + 256
    b = rb[idx].astype(np.float32)
    valid = np.where(qq >= 64, kk >= 64, kk < 576)
    return np.where(valid, b, np.float32(-30000.0)).reshape(5, 128, 128)


def run_model(inp, B, S, D, CAP):
    T = B * S
    NC = NCORES
    Tc = T // NC
    f = lambda n: np.asarray(inp[n], dtype=np.float32)
    x = f("x").reshape(T, D)
    w_in = _c(f("w_in")[0])
    cs1 = _feat_layout(f("norm1_w")[0], D)
    nc = build_proj(Tc, D, PROJ_COLS, True, False)
    maps = [{"aT": _c(x[c * Tc:(c + 1) * Tc].T), "W": w_in, "a": _c(x[c * Tc:(c + 1) * Tc]), "cs": cs1} for c in range(NC)]
    p = np.concatenate([r["out"] for r in _run(nc, maps)], 0)
    NP = 2 * B
    pairs = [[(b, 2 * c + hh) for b in range(B) for hh in range(2)] for c in range(NC)]
    rb = f("rel_bias")[0]
    nc = build_attn(NP, S)
    maps = []
    for c in range(NC):
        qT = np.stack([p[b * S:(b + 1) * S, OFF_ATT + h * HD:OFF_ATT + (h + 1) * HD].T for (b, h) in pairs[c]])
        kT = np.stack([p[b * S:(b + 1) * S, AW + h * HD:AW + (h + 1) * HD].T for (b, h) in pairs[c]])
        v = np.stack([p[b * S:(b + 1) * S, 2 * AW + h * HD:2 * AW + (h + 1) * HD] for (b, h) in pairs[c]])
        bT = np.stack([_make_biasT(rb[h]) for (b, h) in pairs[c]])
        maps.append({"qT": _c(qT), "kT": _c(kT), "v": _c(v), "qw": _c(f("q_norm_w")[0].reshape(128, 1)), "kw": _c(f("k_norm_w")[0].reshape(128, 1)),
                     "biasT": _c(bT)})
    res = _run(nc, maps)
    y = np.zeros((T, 2 * AW), np.float32)
    for c in range(NC):
        for i, (b, h) in enumerate(pairs[c]):
            y[b * S:(b + 1) * S, h * HD:(h + 1) * HD] = res[c]["ya"][i]
    cwf = f("conv_w")[0]
    nc = build_dnpre(NP, S)
    maps = []
    lay = lambda a: a.reshape(S // 128, 128).T
    for c in range(NC):
        pT = np.stack([np.stack([p[b * S:(b + 1) * S, OFF_DN_QKV + cc * AW + h * HD:OFF_DN_QKV + cc * AW + (h + 1) * HD].T for cc in range(3)])
                       for (b, h) in pairs[c]])
        cw = np.stack([np.stack([cwf[:, cc * AW + h * HD:cc * AW + (h + 1) * HD].T for cc in range(3)]) for (b, h) in pairs[c]])
        pb = np.stack([lay(p[b * S:(b + 1) * S, OFF_DN_BETA + h]) for (b, h) in pairs[c]])
        pg = np.stack([lay(p[b * S:(b + 1) * S, OFF_DN_DECAY + h]) for (b, h) in pairs[c]])
        al = np.stack([np.full((128, 1), f("a_log")[0, h], np.float32) for (b, h) in pairs[c]])
        db = np.stack([np.full((128, 1), f("dt_bias")[0, h], np.float32) for (b, h) in pairs[c]])
        maps.append({"pT": _c(pT), "cw": _c(cw), "pb": _c(pb), "pg": _c(pg), "alog": _c(al), "dtb": _c(db)})
    res3 = _run(nc, maps)
    NCH = S // 64
    nc = build_dn(NP, S)
    maps = []
    for c in range(NC):
        oT = res3[c]["oT"]
        tok = lambda a: a.transpose(0, 2, 1).reshape(NP, S)
        chl = lambda a: a.transpose(0, 2, 1).reshape(NP, NCH, 64, 128).transpose(0, 2, 1, 3)
        sl = lambda a: tok(a).reshape(NP, NCH, 64).transpose(0, 2, 1)
        maps.append({"qT": _c(oT[:, 0]), "kT": _c(oT[:, 1]), "kc": _c(chl(oT[:, 1])), "vc": _c(chl(oT[:, 2])),
                     "beta": _c(sl(res3[c]["obeta"])), "g": _c(sl(res3[c]["og"]))})
    res4 = _run(nc, maps)
    o_full = np.zeros((T, AW), np.float32)
    for c in range(NC):
        for i, (b, h) in enumerate(pairs[c]):
            o_full[b * S:(b + 1) * S, h * HD:(h + 1) * HD] = res4[c]["o"][i]
    nc = build_ogate(Tc, NH)
    onw = _c(np.tile(f("o_norm_w")[0][None, :], (128, 1)))
    maps = [{"o": _c(o_full[c * Tc:(c + 1) * Tc]), "z": _c(p[c * Tc:(c + 1) * Tc, OFF_DN_GATE:OFF_DN_GATE + AW]), "onw": onw} for c in range(NC)]
    y[:, AW:] = np.concatenate([r["yb"] for r in _run(nc, maps)], 0)
    del p
    w_out = _c(f("w_out")[0])
    nc = build_proj(Tc, 2 * AW, D, False, True)
    maps = [{"aT": _c(y[c * Tc:(c + 1) * Tc].T), "W": w_out, "res": _c(x[c * Tc:(c + 1) * Tc])} for c in range(NC)]
    x1 = np.concatenate([r["out"] for r in _run(nc, maps)], 0)
    n2 = f("norm2_w")[0]
    Wr = _c(np.concatenate([f("w_group")[0], f("w_router")[0]], 1))
    iot = np.zeros((128, 16), np.float32); iot[:, 0:8] = np.arange(8) * 8; iot[:, 8:16] = np.arange(8)
    nc = build_router(Tc, D)
    maps = [{"a": _c(x1[c * Tc:(c + 1) * Tc]), "aT": _c(x1[c * Tc:(c + 1) * Tc].T), "cs": _feat_layout(n2, D), "csb": _c(np.tile(n2[None, :], (128, 1))),
             "Wr": Wr, "iot": iot} for c in range(NC)]
    res6 = _run(nc, maps)
    h2 = np.concatenate([r["h2"] for r in res6], 0)
    route = np.concatenate([r["route"] for r in res6], 0)
    eid = np.rint(route[:, 0:2]).astype(np.int64)
    flat_e = eid.reshape(-1)
    flat_t = np.repeat(np.arange(T), 2)
    order = np.argsort(flat_e, kind="stable")
    counts = np.bincount(flat_e, minlength=NEXP)
    if counts.max() > CAP:
        raise RuntimeError("expert capacity exceeded: %d > %d" % (counts.max(), CAP))
    starts = np.cumsum(counts) - counts
    rank = np.arange(2 * T) - starts[flat_e[order]]
    slot_sorted = flat_e[order] * CAP + rank
    slot = np.empty(2 * T, np.int64); slot[order] = slot_sorted
    src = np.full(NEXP * CAP, -1, np.int64); src[slot_sorted] = flat_t[order]
    NE = NEXP // NC
    nc = build_experts(D, NE, CAP)
    maps = []
    wgf, wuf, wdf = inp["w_gate"], inp["w_up"], inp["w_down"]
    for c in range(NC):
        s_c = src[c * NE * CAP:(c + 1) * NE * CAP]
        Xc = np.zeros((NE * CAP, D), np.float32)
        m = s_c >= 0
        Xc[m] = h2[s_c[m]]
        maps.append({"XT": _c(Xc.T), "wg": _c(np.asarray(wgf[0, c * NE:(c + 1) * NE])), "wu": _c(np.asarray(wuf[0, c * NE:(c + 1) * NE])),
                     "wd": _c(np.asarray(wdf[0, c * NE:(c + 1) * NE]))})
    Yall = np.concatenate([r["Y"] for r in _run(nc, maps)], 0)
    slot2 = slot.reshape(T, 2)
    nc = build_combine(Tc, D)
    maps = []
    for c in range(NC):
        rows = slice(c * Tc, (c + 1) * Tc)
        maps.append({"x1": _c(x1[rows]), "Y0": _c(Yall[slot2[rows, 0]]), "Y1": _c(Yall[slot2[rows, 1]]), "gt": _c(route[rows, 2:4])})
    out = np.concatenate([r["out"] for r in _run(nc, maps)], 0)
    return out.reshape(B, S, D)


def kernel(**inputs):
    return run_model(inputs, 2, 8192, 4096, 768)
```

```python
import contextlib
import numpy as np
import concourse.bass as bass
import concourse.mybir as mybir

F32 = mybir.dt.float32
BF16 = mybir.dt.bfloat16
I32 = mybir.dt.int32
ALU = mybir.AluOpType
AF = mybir.ActivationFunctionType
AX = mybir.AxisListType

ENGS = ("pe", "act", "dve", "pool", "sp")


class Buf:
    _n = 0
    _all = []

    def __init__(self, name=None):
        Buf._n += 1
        self.name = name or f"b{Buf._n}"
        self.last_write = None
        self.readers = []
        self.dsem = f"d_{self.name}_{Buf._n}"
        self.dcnt = 0
        self.excl = False
        Buf._all.append(self)


class Prog:
    def __init__(self, nc, same_engine_sync=True):
        self.nc = nc
        self.q = {e: [] for e in ENGS}
        self.cnt = {e: 0 for e in ENGS}
        self.waited = {e: {} for e in ENGS}
        self.semkeys = {}
        self.same = same_engine_sync
        Buf._all = []

    def _deps(self, reads, writes):
        deps = []
        for b in reads:
            if b.last_write is not None:
                deps.append(b.last_write)
        for b in writes:
            if b.last_write is not None:
                deps.append(b.last_write)
            deps.extend(b.readers)
        return deps

    def _waits(self, eng, deps):
        waits = []
        best = {}
        for (sk, val, src) in deps:
            if src == eng and (eng == "pe" or not self.same):
                continue
            if self.waited[eng].get(sk, 0) >= val:
                continue
            if best.get(sk, 0) < val:
                best[sk] = val
        for sk, val in best.items():
            self.waited[eng][sk] = val
            waits.append((sk, val))
        return waits

    def op(self, eng, fn, reads=(), writes=()):
        writes = list(writes) + [b for b in reads if b.excl]
        reads = [b for b in reads if not b.excl]
        waits = self._waits(eng, self._deps(reads, writes))
        self.cnt[eng] += 1
        sk = "e_" + eng
        self.semkeys[sk] = None
        tok = (sk, self.cnt[eng], eng)
        self.q[eng].append((waits, fn, (sk, 1)))
        for b in reads:
            b.readers.append(tok)
        for b in writes:
            b.last_write = tok
            b.readers = []
        return tok

    def dma(self, eng, fn, sbuf, reads=(), writes=()):
        deps = self._deps(reads, writes)
        if sbuf.dcnt > 0:
            deps.append((sbuf.dsem, 16 * sbuf.dcnt, "dma"))
        waits = self._waits(eng, deps)
        sbuf.dcnt += 1
        self.semkeys[sbuf.dsem] = None
        tok = (sbuf.dsem, 16 * sbuf.dcnt, "dma")
        self.q[eng].append((waits, fn, (sbuf.dsem, 16)))
        for b in reads:
            b.readers.append(tok)
        for b in writes:
            b.last_write = tok
            b.readers = []
        return tok

    def barrier_all(self):
        deps = []
        for b in Buf._all:
            if b.last_write is not None:
                deps.append(b.last_write)
            deps.extend(b.readers)
        for e in ENGS:
            if self.cnt[e] > 0:
                deps.append(("e_" + e, self.cnt[e], "all"))
        for e in ENGS:
            waits = self._waits(e, [d for d in deps if not (d[0] == "e_" + e)])
            if waits:
                self.q[e].append((waits, None, None))

    def final_wait(self, eng, bufs):
        deps = []
        for b in bufs:
            if b.last_write is not None:
                deps.append(b.last_write)
            deps.extend(b.readers)
        waits = self._waits(eng, deps)
        self.q[eng].append((waits, None, None))

    def emit(self):
        nc = self.nc
        with contextlib.ExitStack() as st:
            sems = {}
            for sk in self.semkeys:
                sems[sk] = st.enter_context(nc.semaphore(sk))
            block = st.enter_context(nc.Block())
            engmap = {"pe": block.tensor, "act": block.scalar, "dve": block.vector,
                      "pool": block.gpsimd, "sp": block.sync}

            def mk(eng):
                def body(e):
                    for waits, fn, inc in self.q[eng]:
                        for sk, val in waits:
                            e.wait_ge(sems[sk], val)
                        if fn is not None:
                            ins = fn(e)
                            ins.then_inc(sems[inc[0]], inc[1])
                return body

            for eng in ENGS:
                if self.q[eng]:
                    engmap[eng](mk(eng))
        print("PROG: insts", {e: len(self.q[e]) for e in ENGS}, "sems", len(self.semkeys))

from concourse.bass_utils import run_bass_kernel_spmd

NCORES = 8


def new_nc():
    return bass.Bass("TRN2", target_bir_lowering=False)


def dram_in(nc, name, shape, dt=F32):
    return nc.dram_tensor(name, list(shape), dt, kind="ExternalInput").ap()


def dram_out(nc, name, shape, dt=F32):
    return nc.dram_tensor(name, list(shape), dt, kind="ExternalOutput").ap()


def build_proj(T, D, N, norm, has_res, eps=1e-6, TG=1024):
    nc = new_nc()
    KT = D // 128
    TG = min(TG, T)
    aT = dram_in(nc, "aT", [D, T])
    W = dram_in(nc, "W", [D, N])
    if norm:
        a = dram_in(nc, "a", [T, D])
        cs = dram_in(nc, "cs", [128, KT])
    if has_res:
        res = dram_in(nc, "res", [T, N])
    out = dram_out(nc, "out", [T, N])
    P = Prog(nc)
    A = nc.alloc_sbuf_tensor
    hT = A("hT", [128, KT, TG], BF16); b_hT = [Buf("hT%d" % i) for i in range(KT)]
    stg = [A("stg%d" % i, [128, TG], F32) for i in range(2)]; b_stg = [Buf("stg%d" % i) for i in range(2)]
    NQ = 4 if KT % 4 == 0 else 1
    KQ = KT // NQ
    wb = [A("wb%d" % i, [128, KT, 512], BF16) for i in range(2)]
    b_wb = [[Buf("wb%d_%d" % (i, q)) for q in range(NQ)] for i in range(2)]
    ost = [A("ost%d" % i, [128, 512], F32) for i in range(3)]; b_ost = [Buf("ost%d" % i) for i in range(3)]
    ps = [nc.alloc_psum_tensor("ps%d" % i, [128, 512], F32) for i in range(2)]; b_ps = [Buf("ps%d" % i) for i in range(2)]
    for b in b_ps:
        b.excl = True
    nsub_all = T // 128
    rstd = A("rstd", [128, nsub_all], F32); b_rstd = Buf("rstd")
    if norm:
        csb = A("csb", [128, KT], F32); b_cs = Buf("cs")
        P.dma("sp", lambda e: e.dma_start(out=csb[:], in_=cs), b_cs, writes=[b_cs])
        at = [A("at%d" % i, [128, D], F32) for i in range(2)]; b_at = [Buf("at%d" % i) for i in range(2)]
        junk = A("junk", [128, D], BF16); b_junk = Buf("junk")
        for s in range(nsub_all):
            i = s % 2
            P.dma("sp", (lambda i, s: lambda e: e.dma_start(out=at[i][:], in_=a[s * 128:(s + 1) * 128, :]))(i, s), b_at[i], writes=[b_at[i]])
            P.op("act", (lambda i, s: lambda e: e.activation(junk[:], at[i][:], AF.Square, accum_out=rstd[:, s:s + 1]))(i, s),
                 reads=[b_at[i]], writes=[b_junk, b_rstd])
        P.op("act", lambda e: e.activation(rstd[:], rstd[:], AF.Sqrt, bias=eps, scale=1.0 / D), reads=[b_rstd], writes=[b_rstd])
        P.op("dve", lambda e: e.reciprocal(rstd[:], rstd[:]), reads=[b_rstd], writes=[b_rstd])
    nchunks = (N + 511) // 512
    wcount = 0
    ocount = 0
    pcount = 0
    for g in range(T // TG):
        t0 = g * TG
        for kt in range(KT):
            i = kt % 2
            P.dma("sp", (lambda i, kt, t0: lambda e: e.dma_start(out=stg[i][:], in_=aT[kt * 128:(kt + 1) * 128, t0:t0 + TG]))(i, kt, t0),
                  b_stg[i], writes=[b_stg[i]])
            if norm:
                P.op("act", (lambda i, kt: lambda e: e.activation(hT[:, kt, :], stg[i][:], AF.Identity, scale=csb[:, kt:kt + 1]))(i, kt),
                     reads=[b_stg[i], b_cs], writes=[b_hT[kt]])
            else:
                P.op("act", (lambda i, kt: lambda e: e.copy(hT[:, kt, :], stg[i][:]))(i, kt), reads=[b_stg[i]], writes=[b_hT[kt]])
        for n in range(nchunks):
            c0 = n * 512
            cw = min(512, N - c0)
            wi = wcount % 2
            wcount += 1
            Wr = W.rearrange("(kt p) n -> p kt n", p=128)
            for q in range(NQ):
                P.dma("pool", (lambda wi, q, c0, cw: lambda e: e.dma_start(out=wb[wi][:, q * KQ:(q + 1) * KQ, 0:cw],
                                                                            in_=Wr[:, q * KQ:(q + 1) * KQ, c0:c0 + cw]))(wi, q, c0, cw),
                      b_wb[wi][q], writes=[b_wb[wi][q]])
            for s in range(TG // 128):
                sg = g * (TG // 128) + s
                pi = pcount % 2
                pcount += 1
                for kt in range(KT):
                    P.op("pe", (lambda pi, kt, s, wi, cw: lambda e: e.matmul(ps[pi][:, 0:cw], hT[:, kt, s * 128:(s + 1) * 128], wb[wi][:, kt, 0:cw],
                                                                               start=(kt == 0), stop=(kt == KT - 1)))(pi, kt, s, wi, cw),
                         reads=[b_hT[kt], b_wb[wi][kt // KQ]], writes=[b_ps[pi]])
                oi = ocount % 3
                ocount += 1
                r0 = t0 + s * 128
                if norm:
                    P.op("act", (lambda oi, pi, cw, sg: lambda e: e.activation(ost[oi][:, 0:cw], ps[pi][:, 0:cw], AF.Identity, scale=rstd[:, sg:sg + 1]))(oi, pi, cw, sg),
                         reads=[b_ps[pi], b_rstd], writes=[b_ost[oi]])
                elif has_res:
                    P.dma("sp", (lambda oi, r0, c0, cw: lambda e: e.dma_start(out=ost[oi][:, 0:cw], in_=res[r0:r0 + 128, c0:c0 + cw]))(oi, r0, c0, cw),
                          b_ost[oi], writes=[b_ost[oi]])
                    P.op("dve", (lambda oi, pi, cw: lambda e: e.tensor_tensor(ost[oi][:, 0:cw], ps[pi][:, 0:cw], ost[oi][:, 0:cw], op=ALU.add))(oi, pi, cw),
                         reads=[b_ps[pi], b_ost[oi]], writes=[b_ost[oi]])
                else:
                    P.op("act", (lambda oi, pi, cw: lambda e: e.copy(ost[oi][:, 0:cw], ps[pi][:, 0:cw]))(oi, pi, cw), reads=[b_ps[pi]], writes=[b_ost[oi]])
                P.dma("sp", (lambda oi, r0, c0, cw: lambda e: e.dma_start(out=out[r0:r0 + 128, c0:c0 + cw], in_=ost[oi][:, 0:cw]))(oi, r0, c0, cw),
                      b_ost[oi], reads=[b_ost[oi]])
    P.final_wait("sp", b_ost)
    P.emit()
    return nc


class K:
    def __init__(self, nc, same=True):
        self.nc = nc
        self.P = Prog(nc, same_engine_sync=same)

    def o(self, eng, method, *args, r=(), w=(), **kw):
        return self.P.op(eng, lambda e: getattr(e, method)(*args, **kw), reads=r, writes=w)

    def d(self, eng, out, in_, sb, r=(), w=()):
        return self.P.dma(eng, lambda e: e.dma_start(out=out, in_=in_), sb, reads=r, writes=w)

    def sb(self, name, shape, dt=F32):
        st = getattr(self, "_stack", None)
        if st is not None:
            return st.enter_context(self.nc.sbuf_tensor(name, list(shape), dt)), Buf(name)
        return self.nc.alloc_sbuf_tensor(name, list(shape), dt), Buf(name)

    def phase_begin(self):
        self._stack = contextlib.ExitStack()

    def phase_end(self):
        self.P.barrier_all()
        self._stack.close()
        self._stack = None
        self.ps_reset()

    def ps(self, name, shape=None, dt=F32):
        if not hasattr(self, "_pspool"):
            self._pspool = []
            self._psidx = 0
        if self._psidx >= len(self._pspool):
            b = Buf("psbank%d" % len(self._pspool))
            b.excl = True
            self._pspool.append((self.nc.alloc_psum_tensor("psbank%d" % len(self._pspool), [128, 512], F32), b))
        r = self._pspool[self._psidx]
        self._psidx += 1
        return r

    def ps_reset(self):
        self._psidx = 0

    def consts(self):
        if getattr(self, "_consts", None) is not None:
            return self._consts
        ones, b1 = self.sb("c_ones", [128, 128]); ident, b2 = self.sb("c_ident", [128, 128])
        ui, b3 = self.sb("c_ui", [64, 64]); us, b4 = self.sb("c_us", [64, 64])
        self.o("pool", "memset", ones[:], 1.0, w=[b1])
        self.o("pool", "memset", ident[:], 1.0, w=[b2])
        self.o("pool", "affine_select", out=ident[:], in_=ident[:], pattern=[[-1, 128]], compare_op=ALU.is_equal, fill=0.0,
               base=0, channel_multiplier=1, r=[b2], w=[b2])
        self.o("pool", "memset", ui[:], 1.0, w=[b3])
        self.o("pool", "memset", us[:], 1.0, w=[b4])
        self.o("pool", "affine_select", out=ui[:], in_=ui[:], pattern=[[1, 64]], compare_op=ALU.is_ge, fill=0.0,
               base=0, channel_multiplier=-1, r=[b3], w=[b3])
        self.o("pool", "affine_select", out=us[:], in_=us[:], pattern=[[1, 64]], compare_op=ALU.is_gt, fill=0.0,
               base=0, channel_multiplier=-1, r=[b4], w=[b4])
        self._consts = ((ones, b1), (ident, b2), (ui, b3), (us, b4))
        return self._consts

    def barrier(self, bufs, name):
        self.P.final_wait("sp", bufs)
        b = Buf(name)
        self.P.op("sp", lambda e: e.nop(), writes=[b])
        return b

    def finish(self, eng, bufs):
        self.P.final_wait(eng, bufs)
        self.P.emit()
        return self.nc


def build_attn(NP, S, eps=1e-6):
    nc = new_nc()
    k = K(nc)
    qT = dram_in(nc, "qT", [NP, 128, S]); kT = dram_in(nc, "kT", [NP, 128, S]); v = dram_in(nc, "v", [NP, S, 128])
    qw = dram_in(nc, "qw", [128, 1]); kw = dram_in(nc, "kw", [128, 1]); biasT = dram_in(nc, "biasT", [NP, 5, 128, 128])
    ya = dram_out(nc, "ya", [NP, S, 128])
    (ones, b_ones), _, _, _ = k.consts()
    NB = S // 128
    PW = min(512, S)
    raw, b_raw = k.sb("raw", [128, S])
    qn, b_qn = k.sb("qn", [128, S], BF16); kn, b_kn = k.sb("kn", [128, S], BF16)
    sq = [k.sb("sq%d" % i, [128, PW]) for i in range(2)]; rr = [k.sb("rr%d" % i, [128, PW]) for i in range(2)]
    v1, b_v1 = k.sb("v1", [128, NB, 132], BF16)
    bT, b_bT = k.sb("bT", [128, 5, 128])
    wq, b_wq = k.sb("wq", [128, 1]); wk, b_wk = k.sb("wk", [128, 1])
    tS = [k.sb("tS%d" % i, [128, 640]) for i in range(2)]
    E = [k.sb("E%d" % i, [128, 640], BF16) for i in range(2)]
    rc, b_rc = k.sb("rc", [128, 1])
    ost = [k.sb("aost%d" % i, [128, 128]) for i in range(2)]
    psA = [k.ps("psSA%d" % i) for i in range(2)]; psB = [k.ps("psSB%d" % i) for i in range(2)]
    psO = [k.ps("psO%d" % i) for i in range(2)]
    psN = psO
    k.d("sp", wq[:], qw, b_wq, w=[b_wq]); k.d("sp", wk[:], kw, b_wk, w=[b_wk])
    k.o("pool", "memset", v1[:], 1.0, w=[b_v1])
    scale = 128.0 ** -0.5
    pc = 0
    for p in range(NP):
        for (src, dst, b_dst, wt, b_wt) in ((qT, qn, b_qn, wq, b_wq), (kT, kn, b_kn, wk, b_wk)):
            k.d("sp", raw[:], src[p], b_raw, w=[b_raw])
            for c0 in range(0, S, PW):
                (sq_, b_sq), (rr_, b_rr), (pn, b_pn) = sq[pc % 2], rr[pc % 2], psN[pc % 2]
                pc += 1
                k.o("act", "activation", sq_[:], raw[:, c0:c0 + PW], AF.Square, r=[b_raw], w=[b_sq])
                k.o("pe", "matmul", pn[:, 0:PW], ones[:], sq_[:], start=True, stop=True, r=[b_ones, b_sq], w=[b_pn])
                k.o("act", "activation", rr_[:], pn[:, 0:PW], AF.Sqrt, bias=eps, scale=1.0 / 128, r=[b_pn], w=[b_rr])
                k.o("dve", "reciprocal", rr_[:], rr_[:], r=[b_rr], w=[b_rr])
                k.o("dve", "scalar_tensor_tensor", dst[:, c0:c0 + PW], raw[:, c0:c0 + PW], wt[:, 0:1], rr_[:], op0=ALU.mult, op1=ALU.mult,
                    r=[b_raw, b_wt, b_rr], w=[b_dst])
        vr = v[p].rearrange("(n p) d -> p n d", p=128)
        for n0 in range(0, NB, 8):
            n1 = min(NB, n0 + 8)
            k.d("pool", v1[:, n0:n1, 0:128], vr[:, n0:n1, :], b_v1, w=[b_v1])
        k.d("sp", bT[:], biasT[p].rearrange("j k q -> k j q"), b_bT, w=[b_bT])
        bTf = bT[:].rearrange("k j q -> k (j q)")

        def emit_S(qb):
            (pa, b_pa), (pb_, b_pb) = psA[qb % 2], psB[qb % 2]
            for kt in range(max(0, qb - 4), qb + 1):
                j = kt - (qb - 4)
                dst_, bd = (pa[:, j * 128:(j + 1) * 128], b_pa) if j < 4 else (pb_[:, 0:128], b_pb)
                k.o("pe", "matmul", dst_, kn[:, kt * 128:(kt + 1) * 128], qn[:, qb * 128:(qb + 1) * 128], start=True, stop=True,
                    r=[b_kn, b_qn], w=[bd])

        emit_S(0)
        for qb in range(NB):
            if qb + 1 < NB:
                emit_S(qb + 1)
            (pa, b_pa), (pb_, b_pb) = psA[qb % 2], psB[qb % 2]
            (t_, b_t), (e_, b_e) = tS[qb % 2], E[qb % 2]
            po, b_po = psO[qb % 2]
            jmin = max(0, 4 - qb)
            if jmin < 4:
                k.o("dve", "scalar_tensor_tensor", t_[:, jmin * 128:512], pa[:, jmin * 128:512], scale, bTf[:, jmin * 128:512], op0=ALU.mult, op1=ALU.add,
                    r=[b_pa, b_bT], w=[b_t])
            k.o("dve", "scalar_tensor_tensor", t_[:, 512:640], pb_[:, 0:128], scale, bTf[:, 512:640], op0=ALU.mult, op1=ALU.add,
                r=[b_pb, b_bT], w=[b_t])
            k.o("act", "activation", e_[:, jmin * 128:640], t_[:, jmin * 128:640], AF.Exp, r=[b_t], w=[b_e])
            kts = list(range(max(0, qb - 4), qb + 1))
            for idx, kt in enumerate(kts):
                j = kt - (qb - 4)
                k.o("pe", "matmul", po[:, 0:129], e_[:, j * 128:(j + 1) * 128], v1[:, kt, 0:129], start=(idx == 0), stop=(idx == len(kts) - 1),
                    r=[b_e, b_v1], w=[b_po])
            os_, b_os = ost[qb % 2]
            k.o("dve", "reciprocal", rc[:], po[:, 128:129], r=[b_po], w=[b_rc])
            k.o("act", "activation", os_[:], po[:, 0:128], AF.Identity, scale=rc[:, 0:1], r=[b_po, b_rc], w=[b_os])
            k.d("sp", ya[p, qb * 128:(qb + 1) * 128, :], os_[:], b_os, r=[b_os])
    return k.finish("sp", [b for (_, b) in ost])


def build_dnpre(NP, S, eps=1e-6):
    nc = new_nc()
    k = K(nc)
    pT = dram_in(nc, "pT", [NP, 3, 128, S]); cw = dram_in(nc, "cw", [NP, 3, 128, 4])
    pb = dram_in(nc, "pb", [NP, 128, S // 128]); pg = dram_in(nc, "pg", [NP, 128, S // 128])
    alog = dram_in(nc, "alog", [NP, 128, 1]); dtb = dram_in(nc, "dtb", [NP, 128, 1])
    oT = dram_out(nc, "oT", [NP, 3, 128, S]); obeta = dram_out(nc, "obeta", [NP, 128, S // 128]); og = dram_out(nc, "og", [NP, 128, S // 128])
    (ones, b_ones), _, _, _ = k.consts()
    PW = min(512, S)
    xin = [k.sb("xin%d" % i, [128, S + 3]) for i in range(2)]
    y = [k.sb("y%d" % i, [128, S]) for i in range(2)]
    cwt, b_cwt = k.sb("cwt", [128, 4])
    sqs = [k.sb("sq%d" % i, [128, PW]) for i in range(3)]; rrs = [k.sb("rr%d" % i, [128, PW]) for i in range(3)]
    psNs = [k.ps("psN%d" % i) for i in range(3)]
    pcn = 0
    sm = [k.sb("sm%d" % i, [128, S // 128]) for i in range(4)]
    al, b_al = k.sb("al", [128, 1]); db, b_db = k.sb("db", [128, 1])
    cnt = 0
    for i in range(2):
        k.o("pool", "memset", xin[i][0][:, 0:3], 0.0, w=[xin[i][1]])
    for p in range(NP):
        for c in range(3):
            xi, b_xi = xin[cnt % 2]; yy, b_y = y[cnt % 2]
            cnt += 1
            k.d("sp", xi[:, 3:S + 3], pT[p, c], b_xi, w=[b_xi])
            k.d("sp", cwt[:], cw[p, c], b_cwt, w=[b_cwt])
            k.o("dve", "tensor_scalar", yy[:], xi[:, 0:S], cwt[:, 0:1], None, op0=ALU.mult, r=[b_xi, b_cwt], w=[b_y])
            for t in range(1, 4):
                k.o("dve", "scalar_tensor_tensor", yy[:], xi[:, t:S + t], cwt[:, t:t + 1], yy[:], op0=ALU.mult, op1=ALU.add,
                    r=[b_xi, b_cwt, b_y], w=[b_y])
            k.o("act", "activation", yy[:], yy[:], AF.Silu, r=[b_y], w=[b_y])
            if c < 2:
                sc = (128.0 ** -0.5) if c == 0 else 1.0
                for c0 in range(0, S, PW):
                    (sq, b_sq), (rr, b_rr), (psN, b_psN) = sqs[pcn % 3], rrs[pcn % 3], psNs[pcn % 3]
                    pcn += 1
                    k.o("act", "activation", sq[:], yy[:, c0:c0 + PW], AF.Square, r=[b_y], w=[b_sq])
                    k.o("pe", "matmul", psN[:, 0:PW], ones[:], sq[:], start=True, stop=True, r=[b_ones, b_sq], w=[b_psN])
                    k.o("act", "activation", rr[:], psN[:, 0:PW], AF.Sqrt, bias=eps, scale=1.0, r=[b_psN], w=[b_rr])
                    k.o("dve", "reciprocal", rr[:], rr[:], r=[b_rr], w=[b_rr])
                    k.o("dve", "scalar_tensor_tensor", yy[:, c0:c0 + PW], yy[:, c0:c0 + PW], sc, rr[:], op0=ALU.mult, op1=ALU.mult,
                        r=[b_y, b_rr], w=[b_y])
            k.d("sp", oT[p, c], yy[:], b_y, r=[b_y])
        (s0, b0), (s1, b1), (s2, b2), (s3, b3) = sm
        k.d("sp", s0[:], pb[p], b0, w=[b0]); k.d("sp", s1[:], pg[p], b1, w=[b1])
        k.d("sp", al[:], alog[p], b_al, w=[b_al]); k.d("sp", db[:], dtb[p], b_db, w=[b_db])
        k.o("act", "activation", s2[:], s0[:], AF.Sigmoid, r=[b0], w=[b2])
        k.d("sp", obeta[p], s2[:], b2, r=[b2])
        k.o("act", "activation", s3[:], s1[:], AF.Exp, bias=db[:, 0:1], r=[b1, b_db], w=[b3])
        k.o("act", "activation", s3[:], s3[:], AF.Ln, bias=1.0, r=[b3], w=[b3])
        k.o("act", "activation", al[:], al[:], AF.Exp, r=[b_al], w=[b_al])
        k.o("dve", "tensor_scalar", s3[:], s3[:], al[:, 0:1], -1.0, op0=ALU.mult, op1=ALU.mult, r=[b3, b_al], w=[b3])
        k.d("sp", og[p], s3[:], b3, r=[b3])
    return k.finish("sp", [b for (_, b) in y] + [b for (_, b) in sm])


def build_dn(NP, S, GC=16, NCHAIN=4, same=False):
    nc = new_nc()
    k = K(nc, same=same)
    NCH = S // 64
    GC = min(GC, NCH)
    NCHAIN = min(NCHAIN, NP)
    qT = dram_in(nc, "qT", [NP, 128, S]); kT = dram_in(nc, "kT", [NP, 128, S])
    kc = dram_in(nc, "kc", [NP, 64, NCH, 128]); vc = dram_in(nc, "vc", [NP, 64, NCH, 128])
    beta = dram_in(nc, "beta", [NP, 64, NCH]); g = dram_in(nc, "g", [NP, 64, NCH])
    o = dram_out(nc, "o", [NP, S, 128])
    (ones, b_ones), (ident, b_id), (ui, b_ui), (us, b_us) = k.consts()
    MM = dict(start=True, stop=True)

    class Ch:
        pass

    chains = []
    for ci in range(NCHAIN):
        c_ = Ch()
        sfx = "_%d" % ci
        for nm, shp in (("qTs", [128, GC * 64]), ("kTs", [128, GC * 64]), ("kcs", [64, GC, 128]), ("vcs", [64, GC, 128]), ("bts", [64, NCH]),
                        ("gs", [64, NCH]), ("Gb", [64, 64]), ("Bb", [64, 64]), ("gcs", [64, 1]), ("gl", [128, 1]), ("egc", [64, 1]), ("egl", [128, 1]),
                        ("dec", [64, 1]), ("be", [64, 1]), ("tS", [64, 64]), ("ET", [64, 64]), ("EU", [64, 64]), ("EB", [64, 64]), ("attT", [64, 64]),
                        ("P0", [64, 64]), ("P1", [64, 64]), ("T0", [64, 64]), ("T1", [64, 64]), ("TT", [64, 64]), ("vb", [64, 128]), ("kb", [64, 128]),
                        ("kd", [64, 128]), ("u_s", [64, 128]), ("wT", [128, 64]), ("vn", [64, 128]), ("o2s", [64, 128]), ("ost", [64, GC, 128]),
                        ("St", [128, 128])):
            t_, b_ = k.sb(nm + sfx, shp)
            setattr(c_, nm, t_); setattr(c_, "b_" + nm, b_)
        for nm in ("X0", "X1"):
            t_, b_ = k.ps(nm + sfx)
            setattr(c_, nm, t_); setattr(c_, "b_" + nm, b_)
        chains.append(c_)

    def chain_prog(c_, plist):
        X0, X1 = c_.X0, c_.X1
        bX0, bX1 = c_.b_X0, c_.b_X1
        for p in plist:
            k.d("sp", c_.bts[:], beta[p], c_.b_bts, w=[c_.b_bts]); k.d("sp", c_.gs[:], g[p], c_.b_gs, w=[c_.b_gs])
            k.o("dve", "memset", c_.St[:], 0.0, w=[c_.b_St])
            yield
            for n in range(NCH):
                c = n % GC
                if c == 0:
                    t0 = n * 64
                    k.d("sp", c_.qTs[:], qT[p, :, t0:t0 + GC * 64], c_.b_qTs, w=[c_.b_qTs]); k.d("sp", c_.kTs[:], kT[p, :, t0:t0 + GC * 64], c_.b_kTs, w=[c_.b_kTs])
                    k.d("sp", c_.kcs[:], kc[p, :, n:n + GC, :], c_.b_kcs, w=[c_.b_kcs]); k.d("sp", c_.vcs[:], vc[p, :, n:n + GC, :], c_.b_vcs, w=[c_.b_vcs])
                qTc = c_.qTs[:, c * 64:(c + 1) * 64]; kTc = c_.kTs[:, c * 64:(c + 1) * 64]
                gcol = c_.gs[:, n:n + 1]; bcol = c_.bts[:, n:n + 1]
                k.o("dve", "tensor_scalar", c_.Gb[:], ones[0:64, 0:64], gcol, None, op0=ALU.mult, r=[b_ones, c_.b_gs], w=[c_.b_Gb]); yield
                k.o("dve", "tensor_scalar", c_.Bb[:], ones[0:64, 0:64], bcol, None, op0=ALU.mult, r=[b_ones, c_.b_bts], w=[c_.b_Bb]); yield
                k.o("pe", "matmul", X0[0:64, 0:64], c_.Gb[:], ui[:], r=[c_.b_Gb, b_ui], w=[bX0], **MM)
                k.o("pe", "matmul", X0[0:64, 64:65], ui[:], gcol, r=[b_ui, c_.b_gs], w=[bX0], **MM)
                k.o("pe", "matmul", X0[0:64, 128:192], c_.Bb[:], ident[0:64, 0:64], r=[c_.b_Bb, b_id], w=[bX0], **MM)
                k.o("pe", "matmul", X0[0:128, 192:193], ones[0:64, 0:128], gcol, r=[b_ones, c_.b_gs], w=[bX0], **MM)
                k.o("pe", "matmul", X0[0:64, 256:320], kTc, kTc, r=[c_.b_kTs], w=[bX0], **MM)
                k.o("pe", "matmul", X0[0:64, 320:384], kTc, qTc, r=[c_.b_kTs, c_.b_qTs], w=[bX0], **MM); yield
                k.o("dve", "tensor_copy", c_.gcs[:], X0[0:64, 64:65], r=[bX0], w=[c_.b_gcs])
                k.o("dve", "tensor_copy", c_.gl[:], X0[0:128, 192:193], r=[bX0], w=[c_.b_gl]); yield
                k.o("act", "activation", c_.egc[:], c_.gcs[:], AF.Exp, r=[c_.b_gcs], w=[c_.b_egc])
                k.o("act", "activation", c_.egl[:], c_.gl[:], AF.Exp, r=[c_.b_gl], w=[c_.b_egl])
                k.o("act", "activation", c_.dec[:], c_.gcs[:], AF.Exp, bias=c_.gl[0:64, 0:1], scale=-1.0, r=[c_.b_gcs, c_.b_gl], w=[c_.b_dec])
                k.o("dve", "tensor_scalar", c_.tS[:], X0[0:64, 0:64], c_.gcs[:, 0:1], 0.0, op0=ALU.subtract, op1=ALU.min, r=[bX0, c_.b_gcs], w=[c_.b_tS]); yield
                k.o("act", "activation", c_.ET[:], c_.tS[:], AF.Exp, r=[c_.b_tS], w=[c_.b_ET])
                k.o("dve", "tensor_tensor", c_.be[:], bcol, c_.egc[:], op=ALU.mult, r=[c_.b_bts, c_.b_egc], w=[c_.b_be]); yield
                k.o("dve", "tensor_tensor", c_.EU[:], c_.ET[:], ui[:], op=ALU.mult, r=[c_.b_ET, b_ui], w=[c_.b_EU])
                k.o("dve", "tensor_tensor", c_.EB[:], X0[0:64, 128:192], c_.ET[:], op=ALU.mult, r=[bX0, c_.b_ET], w=[c_.b_EB])
                k.o("dve", "tensor_tensor", c_.EB[:], c_.EB[:], us[:], op=ALU.mult, r=[c_.b_EB, b_us], w=[c_.b_EB]); yield
                k.o("dve", "tensor_tensor", c_.attT[:], X0[0:64, 320:384], c_.EU[:], op=ALU.mult, r=[bX0, c_.b_EU], w=[c_.b_attT])
                k.o("dve", "scalar_tensor_tensor", c_.T0[:], X0[0:64, 256:320], -1.0, c_.EB[:], op0=ALU.mult, op1=ALU.mult, r=[bX0, c_.b_EB], w=[c_.b_T0]); yield
                k.o("pe", "transpose", X1[0:64, 0:64], c_.T0[:], ident[0:64, 0:64], r=[c_.b_T0, b_id], w=[bX1]); yield
                k.o("act", "copy", c_.P0[:], X1[0:64, 0:64], r=[bX1], w=[c_.b_P0])
                k.o("dve", "tensor_tensor", c_.TT[:], c_.T0[:], ident[0:64, 0:64], op=ALU.add, r=[c_.b_T0, b_id], w=[c_.b_TT]); yield
                Pm = [(c_.P0, c_.b_P0), (c_.P1, c_.b_P1)]; PT = [(c_.T0, c_.b_T0), (c_.T1, c_.b_T1)]
                cur = 0
                for lv in range(5):
                    Pc, b_Pc = Pm[cur]; Tc, b_Tc = PT[cur]; Pn, b_Pn = Pm[1 - cur]; Tn, b_Tn = PT[1 - cur]
                    k.o("pe", "matmul", X1[0:64, 64:128], Tc[:], Pc[:], r=[b_Tc, b_Pc], w=[bX1], **MM)
                    if lv < 4:
                        k.o("pe", "matmul", X1[0:64, 128:192], Pc[:], Tc[:], r=[b_Tc, b_Pc], w=[bX1], **MM)
                    yield
                    k.o("act", "copy", Pn[:], X1[0:64, 64:128], r=[bX1], w=[b_Pn])
                    if lv < 4:
                        k.o("dve", "tensor_copy", Tn[:], X1[0:64, 128:192], r=[bX1], w=[b_Tn])
                    yield
                    k.o("pe", "matmul", X1[0:64, 192:256], Pn[:], c_.TT[:], r=[b_Pn, c_.b_TT], w=[bX1], **MM); yield
                    k.o("dve", "tensor_tensor", c_.TT[:], c_.TT[:], X1[0:64, 192:256], op=ALU.add, r=[c_.b_TT, bX1], w=[c_.b_TT]); yield
                    cur = 1 - cur
                k.o("dve", "tensor_scalar", c_.vb[:], c_.vcs[:, c, :], bcol, None, op0=ALU.mult, r=[c_.b_vcs, c_.b_bts], w=[c_.b_vb])
                k.o("dve", "tensor_scalar", c_.kb[:], c_.kcs[:, c, :], c_.be[:, 0:1], None, op0=ALU.mult, r=[c_.b_kcs, c_.b_be], w=[c_.b_kb])
                k.o("act", "activation", c_.kd[:], c_.kcs[:, c, :], AF.Identity, scale=c_.dec[:, 0:1], r=[c_.b_kcs, c_.b_dec], w=[c_.b_kd]); yield
                k.o("pe", "matmul", X1[0:64, 0:128], c_.TT[:], c_.vb[:], r=[c_.b_TT, c_.b_vb], w=[bX1], **MM)
                k.o("pe", "matmul", X1[0:128, 128:192], c_.kb[:], c_.TT[:], r=[c_.b_TT, c_.b_kb], w=[bX1], **MM); yield
                k.o("act", "copy", c_.u_s[:], X1[0:64, 0:128], r=[bX1], w=[c_.b_u_s])
                k.o("dve", "tensor_copy", c_.wT[:], X1[0:128, 128:192], r=[bX1], w=[c_.b_wT]); yield
                k.o("pe", "matmul", X1[0:64, 192:320], c_.wT[:], c_.St[:], r=[c_.b_wT, c_.b_St], w=[bX1], **MM)
                k.o("pe", "matmul", X1[0:64, 320:448], qTc, c_.St[:], r=[c_.b_qTs, c_.b_St], w=[bX1], **MM); yield
                k.o("dve", "tensor_tensor", c_.vn[:], c_.u_s[:], X1[0:64, 192:320], op=ALU.subtract, r=[c_.b_u_s, bX1], w=[c_.b_vn]); yield
                k.o("pe", "matmul", X1[0:64, 0:128], c_.attT[:], c_.vn[:], r=[c_.b_attT, c_.b_vn], w=[bX1], **MM)
                k.o("pe", "matmul", X0[0:128, 384:512], c_.kd[:], c_.vn[:], r=[c_.b_kd, c_.b_vn], w=[bX0], **MM); yield
                k.o("act", "copy", c_.o2s[:], X1[0:64, 0:128], r=[bX1], w=[c_.b_o2s]); yield
                k.o("dve", "scalar_tensor_tensor", c_.ost[:, c, :], X1[0:64, 320:448], c_.egc[:, 0:1], c_.o2s[:], op0=ALU.mult, op1=ALU.add,
                    r=[bX1, c_.b_egc, c_.b_o2s], w=[c_.b_ost])
                k.o("dve", "scalar_tensor_tensor", c_.St[:], c_.St[:], c_.egl[:, 0:1], X0[0:128, 384:512], op0=ALU.mult, op1=ALU.add,
                    r=[c_.b_St, c_.b_egl, bX0], w=[c_.b_St]); yield
                if c == GC - 1:
                    n0 = n - (GC - 1)
                    k.d("sp", o[p, n0 * 64:(n + 1) * 64, :].rearrange("(n c) d -> c n d", c=64), c_.ost[:], c_.b_ost, r=[c_.b_ost])

    plists = [[p for p in range(NP) if p % NCHAIN == ci] for ci in range(NCHAIN)]
    gens = [chain_prog(chains[ci], plists[ci]) for ci in range(NCHAIN)]
    live = list(gens)
    while live:
        for gno in list(live):
            try:
                next(gno)
            except StopIteration:
                live.remove(gno)
    return k.finish("sp", [c_.b_ost for c_ in chains])


def build_ogate(T, H, eps=1e-6):
    nc = new_nc()
    k = K(nc)
    W_ = H * 128
    o = dram_in(nc, "o", [T, W_]); z = dram_in(nc, "z", [T, W_]); onw = dram_in(nc, "onw", [128, 128])
    yb = dram_out(nc, "yb", [T, W_])
    ot = [k.sb("ot%d" % i, [128, W_]) for i in range(2)]; zt = [k.sb("zt%d" % i, [128, W_]) for i in range(2)]
    wt, b_wt = k.sb("wt", [128, 128]); junk, b_junk = k.sb("junk", [128, 128])
    ss, b_ss = k.sb("ss", [128, H])
    k.d("sp", wt[:], onw, b_wt, w=[b_wt])
    for s in range(T // 128):
        (o_, b_o), (z_, b_z) = ot[s % 2], zt[s % 2]
        k.d("sp", o_[:], o[s * 128:(s + 1) * 128, :], b_o, w=[b_o]); k.d("sp", z_[:], z[s * 128:(s + 1) * 128, :], b_z, w=[b_z])
        k.o("act", "activation", z_[:], z_[:], AF.Silu, r=[b_z], w=[b_z])
        for h in range(H):
            k.o("act", "activation", junk[:], o_[:, h * 128:(h + 1) * 128], AF.Square, accum_out=ss[:, h:h + 1], r=[b_o], w=[b_junk, b_ss])
        k.o("act", "activation", ss[:], ss[:], AF.Sqrt, bias=eps, scale=1.0 / 128, r=[b_ss], w=[b_ss])
        k.o("dve", "reciprocal", ss[:], ss[:], r=[b_ss], w=[b_ss])
        for h in range(H):
            sl = slice(h * 128, (h + 1) * 128)
            k.o("dve", "scalar_tensor_tensor", o_[:, sl], o_[:, sl], ss[:, h:h + 1], wt[:], op0=ALU.mult, op1=ALU.mult, r=[b_o, b_ss, b_wt], w=[b_o])
            k.o("dve", "tensor_tensor", o_[:, sl], o_[:, sl], z_[:, sl], op=ALU.mult, r=[b_o, b_z], w=[b_o])
        k.d("sp", yb[s * 128:(s + 1) * 128, :], o_[:], b_o, r=[b_o])
    return k.finish("sp", [b for (_, b) in ot])


def build_router(T, D, eps=1e-6, TG=256):
    nc = new_nc()
    k = K(nc)
    KT = D // 128
    TG = min(TG, T)
    a = dram_in(nc, "a", [T, D]); aT = dram_in(nc, "aT", [D, T]); cs = dram_in(nc, "cs", [128, KT]); csb = dram_in(nc, "csb", [128, D])
    Wr = dram_in(nc, "Wr", [D, 72]); iot = dram_in(nc, "iot", [128, 16])
    h2 = dram_out(nc, "h2", [T, D]); route = dram_out(nc, "route", [T, 4])
    at = [k.sb("at%d" % i, [128, D]) for i in range(2)]
    junk, b_junk = k.sb("junk", [128, D], BF16)
    cst, b_cs = k.sb("cst", [128, KT]); cbt, b_cb = k.sb("cbt", [128, D]); wr, b_wr = k.sb("wr", [128, KT, 72]); io, b_io = k.sb("io", [128, 16])
    stg = [k.sb("stg%d" % i, [128, TG]) for i in range(2)]
    hT, b_hT = k.sb("hT", [128, KT, TG])
    rs, b_rs = k.sb("rs", [128, 1])
    L, b_L = k.sb("L", [128, 72])
    sm, b_sm = k.sb("sm", [128, 16])
    gm, b_gm = k.sb("gm", [128, 8]); ge, b_ge = k.sb("ge", [128, 8]); ig, b_ig = k.sb("ig", [128, 8]); i2, b_i2 = k.sb("i2", [128, 8])
    m1k, b_m1k = k.sb("m1k", [128, 8]); m2k, b_m2k = k.sb("m2k", [128, 8]); tmp, b_tmp = k.sb("tmp", [128, 8])
    rt = [k.sb("rt%d" % i, [128, 4]) for i in range(2)]
    psL, b_psL = k.ps("psL")
    k.d("sp", cst[:], cs, b_cs, w=[b_cs]); k.d("sp", cbt[:], csb, b_cb, w=[b_cb]); k.d("sp", io[:], iot, b_io, w=[b_io])
    Wrr = Wr.rearrange("(kt p) n -> p kt n", p=128)
    for k0 in range(0, KT, 8):
        k1 = min(KT, k0 + 8)
        k.d("sp", wr[:, k0:k1, :], Wrr[:, k0:k1, :], b_wr, w=[b_wr])
    for g in range(T // TG):
        t0 = g * TG
        for kt in range(KT):
            s_, b_s = stg[kt % 2]
            k.d("sp", s_[:], aT[kt * 128:(kt + 1) * 128, t0:t0 + TG], b_s, w=[b_s])
            k.o("act", "activation", hT[:, kt, :], s_[:], AF.Identity, scale=cst[:, kt:kt + 1], r=[b_s, b_cs], w=[b_hT])
        for s in range(TG // 128):
            r0 = t0 + s * 128
            a_, b_a = at[s % 2]
            k.d("sp", a_[:], a[r0:r0 + 128, :], b_a, w=[b_a])
            k.o("act", "activation", junk[:], a_[:], AF.Square, accum_out=rs[:, 0:1], r=[b_a], w=[b_junk, b_rs])
            k.o("act", "activation", rs[:], rs[:], AF.Sqrt, bias=eps, scale=1.0 / D, r=[b_rs], w=[b_rs])
            k.o("dve", "reciprocal", rs[:], rs[:], r=[b_rs], w=[b_rs])
            for kt in range(KT):
                k.o("pe", "matmul", psL[:, 0:72], hT[:, kt, s * 128:(s + 1) * 128], wr[:, kt, :], start=(kt == 0), stop=(kt == KT - 1),
                    r=[b_hT, b_wr], w=[b_psL])
            k.o("act", "activation", L[:], psL[:, 0:72], AF.Identity, scale=rs[:, 0:1], r=[b_psL, b_rs], w=[b_L])
            k.o("dve", "scalar_tensor_tensor", a_[:], a_[:], rs[:, 0:1], cbt[:], op0=ALU.mult, op1=ALU.mult, r=[b_a, b_rs, b_cb], w=[b_a])
            k.d("sp", h2[r0:r0 + 128, :], a_[:], b_a, r=[b_a])
            k.o("dve", "reduce_max", sm[:, 0:1], L[:, 0:8], axis=AX.X, r=[b_L], w=[b_sm])
            k.o("dve", "tensor_scalar", gm[:], L[:, 0:8], sm[:, 0:1], None, op0=ALU.is_equal, r=[b_L, b_sm], w=[b_gm])
            k.o("dve", "tensor_scalar", sm[:, 1:2], sm[:, 0:1], -1.0, None, op0=ALU.mult, r=[b_sm], w=[b_sm])
            k.o("act", "activation", ge[:], L[:, 0:8], AF.Exp, bias=sm[:, 1:2], accum_out=sm[:, 2:3], r=[b_L, b_sm], w=[b_ge, b_sm])
            k.o("dve", "reciprocal", sm[:, 3:4], sm[:, 2:3], r=[b_sm], w=[b_sm])
            k.o("dve", "tensor_scalar", ig[:], L[:, 8:16], gm[:, 0:1], None, op0=ALU.mult, r=[b_L, b_gm], w=[b_ig])
            for gi in range(1, 8):
                k.o("dve", "scalar_tensor_tensor", ig[:], L[:, 8 + gi * 8:16 + gi * 8], gm[:, gi:gi + 1], ig[:], op0=ALU.mult, op1=ALU.add,
                    r=[b_L, b_gm, b_ig], w=[b_ig])
            k.o("dve", "reduce_max", sm[:, 4:5], ig[:], axis=AX.X, r=[b_ig], w=[b_sm])
            k.o("dve", "tensor_scalar", m1k[:], ig[:], sm[:, 4:5], None, op0=ALU.is_equal, r=[b_ig, b_sm], w=[b_m1k])
            k.o("dve", "scalar_tensor_tensor", i2[:], m1k[:], -1e30, ig[:], op0=ALU.mult, op1=ALU.add, r=[b_m1k, b_ig], w=[b_i2])
            k.o("dve", "reduce_max", sm[:, 5:6], i2[:], axis=AX.X, r=[b_i2], w=[b_sm])
            k.o("dve", "tensor_scalar", m2k[:], i2[:], sm[:, 5:6], None, op0=ALU.is_equal, r=[b_i2, b_sm], w=[b_m2k])
            k.o("dve", "tensor_tensor", sm[:, 6:7], sm[:, 4:5], sm[:, 5:6], op=ALU.subtract, r=[b_sm], w=[b_sm])
            k.o("act", "activation", sm[:, 7:8], sm[:, 6:7], AF.Sigmoid, r=[b_sm], w=[b_sm])
            k.o("act", "activation", sm[:, 8:9], sm[:, 6:7], AF.Sigmoid, scale=-1.0, r=[b_sm], w=[b_sm])
            k.o("dve", "tensor_tensor", tmp[:], gm[:], io[:, 0:8], op=ALU.mult, r=[b_gm, b_io], w=[b_tmp])
            k.o("dve", "reduce_sum", sm[:, 9:10], tmp[:], axis=AX.X, r=[b_tmp], w=[b_sm])
            k.o("dve", "tensor_tensor", tmp[:], m1k[:], io[:, 8:16], op=ALU.mult, r=[b_m1k, b_io], w=[b_tmp])
            k.o("dve", "reduce_sum", sm[:, 10:11], tmp[:], axis=AX.X, r=[b_tmp], w=[b_sm])
            k.o("dve", "tensor_tensor", tmp[:], m2k[:], io[:, 8:16], op=ALU.mult, r=[b_m2k, b_io], w=[b_tmp])
            k.o("dve", "reduce_sum", sm[:, 11:12], tmp[:], axis=AX.X, r=[b_tmp], w=[b_sm])
            r_, b_r = rt[s % 2]
            k.o("dve", "tensor_tensor", r_[:, 0:1], sm[:, 9:10], sm[:, 10:11], op=ALU.add, r=[b_sm], w=[b_r])
            k.o("dve", "tensor_tensor", r_[:, 1:2], sm[:, 9:10], sm[:, 11:12], op=ALU.add, r=[b_sm, b_r], w=[b_r])
            k.o("dve", "tensor_tensor", r_[:, 2:3], sm[:, 3:4], sm[:, 7:8], op=ALU.mult, r=[b_sm, b_r], w=[b_r])
            k.o("dve", "tensor_tensor", r_[:, 3:4], sm[:, 3:4], sm[:, 8:9], op=ALU.mult, r=[b_sm, b_r], w=[b_r])
            k.d("sp", route[r0:r0 + 128, :], r_[:], b_r, r=[b_r])
    return k.finish("sp", [b for (_, b) in at] + [b for (_, b) in rt])


def build_experts(D, NE, CAP, DE=512, NS=256):
    nc = new_nc()
    k = K(nc)
    KT = D // 128; HT = DE // 128
    NS = min(NS, CAP)
    XT = dram_in(nc, "XT", [D, NE * CAP]); wg = dram_in(nc, "wg", [NE, D, DE]); wu = dram_in(nc, "wu", [NE, D, DE]); wd = dram_in(nc, "wd", [NE, DE, D])
    Y = dram_out(nc, "Y", [NE * CAP, D])
    NQ = 4 if KT % 4 == 0 else 1
    KQ = KT // NQ
    wgb = [k.nc.alloc_sbuf_tensor("wgb%d" % i, [128, KT, DE], BF16) for i in range(2)]
    wub = [k.nc.alloc_sbuf_tensor("wub%d" % i, [128, KT, DE], BF16) for i in range(2)]
    b_wg = [[Buf("wg%d_%d" % (i, q)) for q in range(NQ)] for i in range(2)]; b_wu = [[Buf("wu%d_%d" % (i, q)) for q in range(NQ)] for i in range(2)]
    wdb = k.nc.alloc_sbuf_tensor("wdb", [128, HT, D], BF16); b_wd = [Buf("wd%d" % h) for h in range(HT)]
    xt = k.nc.alloc_sbuf_tensor("xt", [128, KT, NS], BF16); b_xt = [Buf("xt%d" % q) for q in range(NQ)]
    sg, b_sg = k.sb("sg", [128, NS]); hid, b_hid = k.sb("hid", [128, HT, NS], BF16)
    yst = [k.sb("yst%d" % i, [128, 512]) for i in range(3)]
    psG = [k.ps("psG%d" % i) for i in range(2)]; psU = [k.ps("psU%d" % i) for i in range(2)]; psY = [k.ps("psY%d" % i) for i in range(2)]
    XTr = XT.rearrange("(kt p) s -> p kt s", p=128)
    yc = 0
    for le in range(NE):
        wi = le % 2
        wgr = wg[le].rearrange("(kt p) n -> p kt n", p=128); wur = wu[le].rearrange("(kt p) n -> p kt n", p=128)
        wdr = wd[le].rearrange("(ht p) n -> p ht n", p=128)
        for q in range(NQ):
            k.d("pool", wgb[wi][:, q * KQ:(q + 1) * KQ, :], wgr[:, q * KQ:(q + 1) * KQ, :], b_wg[wi][q], w=[b_wg[wi][q]])
            k.d("pool", wub[wi][:, q * KQ:(q + 1) * KQ, :], wur[:, q * KQ:(q + 1) * KQ, :], b_wu[wi][q], w=[b_wu[wi][q]])
        for h in range(HT):
            k.d("pool", wdb[:, h, :], wdr[:, h, :], b_wd[h], w=[b_wd[h]])
        for sc in range(CAP // NS):
            s0 = le * CAP + sc * NS
            for q in range(NQ):
                k.d("pool", xt[:, q * KQ:(q + 1) * KQ, :], XTr[:, q * KQ:(q + 1) * KQ, s0:s0 + NS], b_xt[q], w=[b_xt[q]])
            for ht in range(HT):
                (pg_, b_pg), (pu_, b_pu) = psG[ht % 2], psU[ht % 2]
                for kt in range(KT):
                    k.o("pe", "matmul", pg_[:, 0:NS], wgb[wi][:, kt, ht * 128:(ht + 1) * 128], xt[:, kt, :], start=(kt == 0), stop=(kt == KT - 1),
                        r=[b_wg[wi][kt // KQ], b_xt[kt // KQ]], w=[b_pg])
                for kt in range(KT):
                    k.o("pe", "matmul", pu_[:, 0:NS], wub[wi][:, kt, ht * 128:(ht + 1) * 128], xt[:, kt, :], start=(kt == 0), stop=(kt == KT - 1),
                        r=[b_wu[wi][kt // KQ], b_xt[kt // KQ]], w=[b_pu])
                k.o("act", "activation", sg[:], pg_[:, 0:NS], AF.Silu, r=[b_pg], w=[b_sg])
                k.o("dve", "tensor_tensor", hid[:, ht, :], sg[:], pu_[:, 0:NS], op=ALU.mult, r=[b_sg, b_pu], w=[b_hid])
            for st in range(NS // 128):
                for cc in range(D // 512):
                    py, b_py = psY[yc % 2]; ys, b_ys = yst[yc % 3]
                    for ht in range(HT):
                        k.o("pe", "matmul", py[:, 0:512], hid[:, ht, st * 128:(st + 1) * 128], wdb[:, ht, cc * 512:(cc + 1) * 512],
                            start=(ht == 0), stop=(ht == HT - 1), r=[b_hid, b_wd[ht]], w=[b_py])
                    if yc % 2 == 0:
                        k.o("act", "copy", ys[:], py[:, 0:512], r=[b_py], w=[b_ys])
                    else:
                        k.o("dve", "tensor_copy", ys[:], py[:, 0:512], r=[b_py], w=[b_ys])
                    k.d("sp", Y[s0 + st * 128:s0 + (st + 1) * 128, cc * 512:(cc + 1) * 512], ys[:], b_ys, r=[b_ys])
                    yc += 1
    return k.finish("sp", [b for (_, b) in yst])


def build_combine(T, D):
    nc = new_nc()
    k = K(nc)
    x1 = dram_in(nc, "x1", [T, D]); Y0 = dram_in(nc, "Y0", [T, D]); Y1 = dram_in(nc, "Y1", [T, D]); gt = dram_in(nc, "gt", [T, 2])
    out = dram_out(nc, "out", [T, D])
    xa = [k.sb("xa%d" % i, [128, D]) for i in range(2)]; ya = [k.sb("ya%d" % i, [128, D]) for i in range(2)]; yb = [k.sb("yb%d" % i, [128, D]) for i in range(2)]
    gg = [k.sb("gg%d" % i, [128, 2]) for i in range(2)]
    for s in range(T // 128):
        i = s % 2
        rows = slice(s * 128, (s + 1) * 128)
        (x_, b_x), (a_, b_a), (b_, b_b), (g_, b_g) = xa[i], ya[i], yb[i], gg[i]
        k.d("sp", x_[:], x1[rows, :], b_x, w=[b_x]); k.d("sp", a_[:], Y0[rows, :], b_a, w=[b_a]); k.d("sp", b_[:], Y1[rows, :], b_b, w=[b_b])
        k.d("sp", g_[:], gt[rows, :], b_g, w=[b_g])
        k.o("dve", "scalar_tensor_tensor", x_[:], a_[:], g_[:, 0:1], x_[:], op0=ALU.mult, op1=ALU.add, r=[b_a, b_g, b_x], w=[b_x])
        k.o("dve", "scalar_tensor_tensor", x_[:], b_[:], g_[:, 1:2], x_[:], op0=ALU.mult, op1=ALU.add, r=[b_b, b_g, b_x], w=[b_x])
        k.d("sp", out[rows, :], x_[:], b_x, r=[b_x])
    return k.finish("sp", [b for (_, b) in xa])


def attn_phase(k, NP, S, qT, kT, v, qw, kw, biasT, ya, pfx, eps=1e-6):
    nc = k.nc
    (ones, b_ones), _, _, _ = k.consts()
    NB = S // 128
    PW = min(512, S)
    raw, b_raw = k.sb(pfx + "raw", [128, S])
    qn, b_qn = k.sb(pfx + "qn", [128, S], BF16); kn, b_kn = k.sb(pfx + "kn", [128, S], BF16)
    sq = [k.sb(pfx + "sq%d" % i, [128, PW]) for i in range(2)]; rr = [k.sb(pfx + "rr%d" % i, [128, PW]) for i in range(2)]
    v1, b_v1 = k.sb(pfx + "v1", [128, NB, 132], BF16)
    bT, b_bT = k.sb(pfx + "bT", [128, 5, 128])
    wq, b_wq = k.sb(pfx + "wq", [128, 1]); wk, b_wk = k.sb(pfx + "wk", [128, 1])
    tS = [k.sb(pfx + "tS%d" % i, [128, 640]) for i in range(2)]
    E = [k.sb(pfx + "E%d" % i, [128, 640], BF16) for i in range(2)]
    rc, b_rc = k.sb(pfx + "rc", [128, 1])
    ost = [k.sb(pfx + "aost%d" % i, [128, 128]) for i in range(2)]
    psA = [k.ps(pfx + "psSA%d" % i) for i in range(2)]; psB = [k.ps(pfx + "psSB%d" % i) for i in range(2)]
    psO = [k.ps(pfx + "psO%d" % i) for i in range(2)]
    psN = psO
    k.d("sp", wq[:], qw, b_wq, w=[b_wq]); k.d("sp", wk[:], kw, b_wk, w=[b_wk])
    k.o("pool", "memset", v1[:], 1.0, w=[b_v1])
    scale = 128.0 ** -0.5
    pc = 0
    for p in range(NP):
        for (src, dst, b_dst, wt, b_wt) in ((qT, qn, b_qn, wq, b_wq), (kT, kn, b_kn, wk, b_wk)):
            k.d("sp", raw[:], src[p], b_raw, w=[b_raw])
            for c0 in range(0, S, PW):
                (sq_, b_sq), (rr_, b_rr), (pn, b_pn) = sq[pc % 2], rr[pc % 2], psN[pc % 2]
                pc += 1
                k.o("act", "activation", sq_[:], raw[:, c0:c0 + PW], AF.Square, r=[b_raw], w=[b_sq])
                k.o("pe", "matmul", pn[:, 0:PW], ones[:], sq_[:], start=True, stop=True, r=[b_ones, b_sq], w=[b_pn])
                k.o("act", "activation", rr_[:], pn[:, 0:PW], AF.Sqrt, bias=eps, scale=1.0 / 128, r=[b_pn], w=[b_rr])
                k.o("dve", "reciprocal", rr_[:], rr_[:], r=[b_rr], w=[b_rr])
                k.o("dve", "scalar_tensor_tensor", dst[:, c0:c0 + PW], raw[:, c0:c0 + PW], wt[:, 0:1], rr_[:], op0=ALU.mult, op1=ALU.mult,
                    r=[b_raw, b_wt, b_rr], w=[b_dst])
        vr = v[p].rearrange("(n p) d -> p n d", p=128)
        for n0 in range(0, NB, 8):
            n1 = min(NB, n0 + 8)
            k.d("pool", v1[:, n0:n1, 0:128], vr[:, n0:n1, :], b_v1, w=[b_v1])
        k.d("sp", bT[:], biasT[p].rearrange("j k q -> k j q"), b_bT, w=[b_bT])
        bTf = bT[:].rearrange("k j q -> k (j q)")

        def emit_S(qb):
            (pa, b_pa), (pb_, b_pb) = psA[qb % 2], psB[qb % 2]
            for kt in range(max(0, qb - 4), qb + 1):
                j = kt - (qb - 4)
                dst_, bd = (pa[:, j * 128:(j + 1) * 128], b_pa) if j < 4 else (pb_[:, 0:128], b_pb)
                k.o("pe", "matmul", dst_, kn[:, kt * 128:(kt + 1) * 128], qn[:, qb * 128:(qb + 1) * 128], start=True, stop=True,
                    r=[b_kn, b_qn], w=[bd])

        emit_S(0)
        for qb in range(NB):
            if qb + 1 < NB:
                emit_S(qb + 1)
            (pa, b_pa), (pb_, b_pb) = psA[qb % 2], psB[qb % 2]
            (t_, b_t), (e_, b_e) = tS[qb % 2], E[qb % 2]
            po, b_po = psO[qb % 2]
            jmin = max(0, 4 - qb)
            if jmin < 4:
                k.o("dve", "scalar_tensor_tensor", t_[:, jmin * 128:512], pa[:, jmin * 128:512], scale, bTf[:, jmin * 128:512], op0=ALU.mult, op1=ALU.add,
                    r=[b_pa, b_bT], w=[b_t])
            k.o("dve", "scalar_tensor_tensor", t_[:, 512:640], pb_[:, 0:128], scale, bTf[:, 512:640], op0=ALU.mult, op1=ALU.add,
                r=[b_pb, b_bT], w=[b_t])
            k.o("act", "activation", e_[:, jmin * 128:640], t_[:, jmin * 128:640], AF.Exp, r=[b_t], w=[b_e])
            kts = list(range(max(0, qb - 4), qb + 1))
            for idx, kt in enumerate(kts):
                j = kt - (qb - 4)
                k.o("pe", "matmul", po[:, 0:129], e_[:, j * 128:(j + 1) * 128], v1[:, kt, 0:129], start=(idx == 0), stop=(idx == len(kts) - 1),
                    r=[b_e, b_v1], w=[b_po])
            os_, b_os = ost[qb % 2]
            k.o("dve", "reciprocal", rc[:], po[:, 128:129], r=[b_po], w=[b_rc])
            k.o("act", "activation", os_[:], po[:, 0:128], AF.Identity, scale=rc[:, 0:1], r=[b_po, b_rc], w=[b_os])
            k.d("sp", ya[p, qb * 128:(qb + 1) * 128, :], os_[:], b_os, r=[b_os])
    return [b for (_, b) in ost]


def dnpre_phase(k, NP, S, pT, cw, pb, pg, alog, dtb, oT, ktok, vtok, obeta, og, pfx, eps=1e-6):
    nc = k.nc
    (ones, b_ones), (ident, b_id), _, _ = k.consts()
    NCH = S // 64
    tks = [k.sb(pfx + "tk%d" % i, [128, 4, 128]) for i in range(2)]
    psT = [k.ps(pfx + "psT%d" % i) for i in range(2)]
    tcn = 0
    PW = min(512, S)
    xin = [k.sb(pfx + "xin%d" % i, [128, S + 3]) for i in range(2)]
    y = [k.sb(pfx + "y%d" % i, [128, S]) for i in range(2)]
    cwt, b_cwt = k.sb(pfx + "cwt", [128, 4])
    sqs = [k.sb(pfx + "sq%d" % i, [128, PW]) for i in range(3)]; rrs = [k.sb(pfx + "rr%d" % i, [128, PW]) for i in range(3)]
    psNs = [k.ps(pfx + "psN%d" % i) for i in range(3)]
    pcn = 0
    sm = [k.sb(pfx + "sm%d" % i, [64, NCH]) for i in range(4)]
    al, b_al = k.sb(pfx + "al", [64, 1]); db, b_db = k.sb(pfx + "db", [64, 1])
    cnt = 0
    for i in range(2):
        k.o("pool", "memset", xin[i][0][:, 0:3], 0.0, w=[xin[i][1]])
    for p in range(NP):
        for c in range(3):
            xi, b_xi = xin[cnt % 2]; yy, b_y = y[cnt % 2]
            cnt += 1
            k.d("sp", xi[:, 3:S + 3], pT[p, c], b_xi, w=[b_xi])
            k.d("sp", cwt[:], cw[p, c], b_cwt, w=[b_cwt])
            k.o("dve", "tensor_scalar", yy[:], xi[:, 0:S], cwt[:, 0:1], None, op0=ALU.mult, r=[b_xi, b_cwt], w=[b_y])
            for t in range(1, 4):
                k.o("dve", "scalar_tensor_tensor", yy[:], xi[:, t:S + t], cwt[:, t:t + 1], yy[:], op0=ALU.mult, op1=ALU.add,
                    r=[b_xi, b_cwt, b_y], w=[b_y])
            k.o("act", "activation", yy[:], yy[:], AF.Silu, r=[b_y], w=[b_y])
            if c < 2:
                sc = (128.0 ** -0.5) if c == 0 else 1.0
                for c0 in range(0, S, PW):
                    (sq, b_sq), (rr, b_rr), (psN, b_psN) = sqs[pcn % 3], rrs[pcn % 3], psNs[pcn % 3]
                    pcn += 1
                    k.o("act", "activation", sq[:], yy[:, c0:c0 + PW], AF.Square, r=[b_y], w=[b_sq])
                    k.o("pe", "matmul", psN[:, 0:PW], ones[:], sq[:], start=True, stop=True, r=[b_ones, b_sq], w=[b_psN])
                    k.o("act", "activation", rr[:], psN[:, 0:PW], AF.Sqrt, bias=eps, scale=1.0, r=[b_psN], w=[b_rr])
                    k.o("dve", "reciprocal", rr[:], rr[:], r=[b_rr], w=[b_rr])
                    k.o("dve", "scalar_tensor_tensor", yy[:, c0:c0 + PW], yy[:, c0:c0 + PW], sc, rr[:], op0=ALU.mult, op1=ALU.mult,
                        r=[b_y, b_rr], w=[b_y])
            if c < 2:
                k.d("sp", oT[p, c], yy[:], b_y, r=[b_y])
            if c >= 1:
                dstT = ktok if c == 1 else vtok
                for b0 in range(0, S // 128, 4):
                    (pt, b_pt), (tk, b_tk) = psT[tcn % 2], tks[tcn % 2]
                    tcn += 1
                    for bb in range(4):
                        k.o("pe", "transpose", pt[:, bb * 128:(bb + 1) * 128], yy[:, (b0 + bb) * 128:(b0 + bb + 1) * 128], ident[:], r=[b_y, b_id], w=[b_pt])
                    k.o("act", "copy", tk[:].rearrange("p n d -> p (n d)"), pt[:, 0:512], r=[b_pt], w=[b_tk])
                    k.d("sp", dstT[p, b0 * 128:(b0 + 4) * 128, :].rearrange("(n p) d -> p n d", p=128), tk[:], b_tk, r=[b_tk])
        (s0, b0), (s1, b1), (s2, b2), (s3, b3) = sm
        k.d("sp", s0[:], pb[p], b0, w=[b0]); k.d("sp", s1[:], pg[p], b1, w=[b1])
        k.d("sp", al[:], alog[p], b_al, w=[b_al]); k.d("sp", db[:], dtb[p], b_db, w=[b_db])
        k.o("act", "activation", s2[:], s0[:], AF.Sigmoid, r=[b0], w=[b2])
        k.d("sp", obeta[p], s2[:], b2, r=[b2])
        k.o("act", "activation", s3[:], s1[:], AF.Exp, bias=db[:, 0:1], r=[b1, b_db], w=[b3])
        k.o("act", "activation", s3[:], s3[:], AF.Ln, bias=1.0, r=[b3], w=[b3])
        k.o("act", "activation", al[:], al[:], AF.Exp, r=[b_al], w=[b_al])
        k.o("dve", "tensor_scalar", s3[:], s3[:], al[:, 0:1], -1.0, op0=ALU.mult, op1=ALU.mult, r=[b3, b_al], w=[b3])
        k.d("sp", og[p], s3[:], b3, r=[b3])
    return [b for (_, b) in y] + [b for (_, b) in sm] + [b for (_, b) in tks]


def dn_phase(k, NP, S, qT, kT, kc, vc, zc, beta, g, onw, yb, dep, pfx, GC=16, NCHAIN=4, eps=1e-6):
    nc = k.nc
    NCH = S // 64
    GC = min(GC, NCH)
    NCHAIN = min(NCHAIN, NP)
    onwt, b_onw = k.sb(pfx + "onwt", [64, 128])
    k.d("sp", onwt[:], onw, b_onw, w=[b_onw])
    (ones, b_ones), (ident, b_id), (ui, b_ui), (us, b_us) = k.consts()
    MM = dict(start=True, stop=True)

    class Ch:
        pass

    chains = []
    for ci in range(NCHAIN):
        c_ = Ch()
        sfx = "_%d" % ci
        for nm, shp in (("qTs", [128, GC * 64]), ("kTs", [128, GC * 64]), ("kcs", [64, GC, 128]), ("vcs", [64, GC, 128]), ("bts", [64, NCH]),
                        ("gs", [64, NCH]), ("Gb", [64, 64]), ("Bb", [64, 64]), ("gcs", [64, 1]), ("gl", [128, 1]), ("egc", [64, 1]), ("egl", [128, 1]),
                        ("dec", [64, 1]), ("be", [64, 1]), ("tS", [64, 64]), ("ET", [64, 64]), ("EU", [64, 64]), ("EB", [64, 64]), ("attT", [64, 64]),
                        ("P0", [64, 64]), ("P1", [64, 64]), ("T0", [64, 64]), ("T1", [64, 64]), ("TT", [64, 64]), ("vb", [64, 128]), ("kb", [64, 128]),
                        ("kd", [64, 128]), ("u_s", [64, 128]), ("wT", [128, 64]), ("vn", [64, 128]), ("o2s", [64, 128]), ("ost", [64, GC, 128]),
                        ("St", [128, 128]), ("zcs", [64, GC, 128]), ("ssq", [64, 1]), ("ojunk", [64, 128])):
            t_, b_ = k.sb(pfx + nm + sfx, shp)
            setattr(c_, nm, t_); setattr(c_, "b_" + nm, b_)
        for nm in ("X0", "X1"):
            t_, b_ = k.ps(pfx + nm + sfx)
            setattr(c_, nm, t_); setattr(c_, "b_" + nm, b_)
        chains.append(c_)

    def chain_prog(c_, plist):
        X0, X1 = c_.X0, c_.X1
        bX0, bX1 = c_.b_X0, c_.b_X1
        for p in plist:
            k.d("sp", c_.bts[:], beta[p], c_.b_bts, r=dep, w=[c_.b_bts]); k.d("sp", c_.gs[:], g[p], c_.b_gs, r=dep, w=[c_.b_gs])
            k.o("dve", "memset", c_.St[:], 0.0, w=[c_.b_St])
            yield
            for n in range(NCH):
                c = n % GC
                if c == 0:
                    t0 = n * 64
                    k.d("sp", c_.qTs[:], qT[p, :, t0:t0 + GC * 64], c_.b_qTs, r=dep, w=[c_.b_qTs]); k.d("sp", c_.kTs[:], kT[p, :, t0:t0 + GC * 64], c_.b_kTs, r=dep, w=[c_.b_kTs])
                    k.d("sp", c_.kcs[:], kc[p][n * 64:(n + GC) * 64, :].rearrange("(n c) d -> c n d", c=64), c_.b_kcs, r=dep, w=[c_.b_kcs]); k.d("sp", c_.vcs[:], vc[p][n * 64:(n + GC) * 64, :].rearrange("(n c) d -> c n d", c=64), c_.b_vcs, r=dep, w=[c_.b_vcs])
                    k.d("sp", c_.zcs[:], zc[p, :, n:n + GC, :], c_.b_zcs, w=[c_.b_zcs])
                    k.o("act", "activation", c_.zcs[:], c_.zcs[:], AF.Silu, r=[c_.b_zcs], w=[c_.b_zcs])
                qTc = c_.qTs[:, c * 64:(c + 1) * 64]; kTc = c_.kTs[:, c * 64:(c + 1) * 64]
                gcol = c_.gs[:, n:n + 1]; bcol = c_.bts[:, n:n + 1]
                k.o("dve", "tensor_scalar", c_.Gb[:], ones[0:64, 0:64], gcol, None, op0=ALU.mult, r=[b_ones, c_.b_gs], w=[c_.b_Gb]); yield
                k.o("dve", "tensor_scalar", c_.Bb[:], ones[0:64, 0:64], bcol, None, op0=ALU.mult, r=[b_ones, c_.b_bts], w=[c_.b_Bb]); yield
                k.o("pe", "matmul", X0[0:64, 0:64], c_.Gb[:], ui[:], r=[c_.b_Gb, b_ui], w=[bX0], **MM)
                k.o("pe", "matmul", X0[0:64, 64:65], ui[:], gcol, r=[b_ui, c_.b_gs], w=[bX0], **MM)
                k.o("pe", "matmul", X0[0:64, 128:192], c_.Bb[:], ident[0:64, 0:64], r=[c_.b_Bb, b_id], w=[bX0], **MM)
                k.o("pe", "matmul", X0[0:128, 192:193], ones[0:64, 0:128], gcol, r=[b_ones, c_.b_gs], w=[bX0], **MM)
                k.o("pe", "matmul", X0[0:64, 256:320], kTc, kTc, r=[c_.b_kTs], w=[bX0], **MM)
                k.o("pe", "matmul", X0[0:64, 320:384], kTc, qTc, r=[c_.b_kTs, c_.b_qTs], w=[bX0], **MM); yield
                k.o("dve", "tensor_copy", c_.gcs[:], X0[0:64, 64:65], r=[bX0], w=[c_.b_gcs])
                k.o("dve", "tensor_copy", c_.gl[:], X0[0:128, 192:193], r=[bX0], w=[c_.b_gl]); yield
                k.o("act", "activation", c_.egc[:], c_.gcs[:], AF.Exp, r=[c_.b_gcs], w=[c_.b_egc])
                k.o("act", "activation", c_.egl[:], c_.gl[:], AF.Exp, r=[c_.b_gl], w=[c_.b_egl])
                k.o("act", "activation", c_.dec[:], c_.gcs[:], AF.Exp, bias=c_.gl[0:64, 0:1], scale=-1.0, r=[c_.b_gcs, c_.b_gl], w=[c_.b_dec])
                k.o("dve", "tensor_scalar", c_.tS[:], X0[0:64, 0:64], c_.gcs[:, 0:1], 0.0, op0=ALU.subtract, op1=ALU.min, r=[bX0, c_.b_gcs], w=[c_.b_tS]); yield
                k.o("act", "activation", c_.ET[:], c_.tS[:], AF.Exp, r=[c_.b_tS], w=[c_.b_ET])
                k.o("dve", "tensor_tensor", c_.be[:], bcol, c_.egc[:], op=ALU.mult, r=[c_.b_bts, c_.b_egc], w=[c_.b_be]); yield
                k.o("dve", "tensor_tensor", c_.EU[:], c_.ET[:], ui[:], op=ALU.mult, r=[c_.b_ET, b_ui], w=[c_.b_EU])
                k.o("dve", "tensor_tensor", c_.EB[:], X0[0:64, 128:192], c_.ET[:], op=ALU.mult, r=[bX0, c_.b_ET], w=[c_.b_EB])
                k.o("dve", "tensor_tensor", c_.EB[:], c_.EB[:], us[:], op=ALU.mult, r=[c_.b_EB, b_us], w=[c_.b_EB]); yield
                k.o("dve", "tensor_tensor", c_.attT[:], X0[0:64, 320:384], c_.EU[:], op=ALU.mult, r=[bX0, c_.b_EU], w=[c_.b_attT])
                k.o("dve", "scalar_tensor_tensor", c_.T0[:], X0[0:64, 256:320], -1.0, c_.EB[:], op0=ALU.mult, op1=ALU.mult, r=[bX0, c_.b_EB], w=[c_.b_T0]); yield
                k.o("pe", "transpose", X1[0:64, 0:64], c_.T0[:], ident[0:64, 0:64], r=[c_.b_T0, b_id], w=[bX1]); yield
                k.o("act", "copy", c_.P0[:], X1[0:64, 0:64], r=[bX1], w=[c_.b_P0])
                k.o("dve", "tensor_tensor", c_.TT[:], c_.T0[:], ident[0:64, 0:64], op=ALU.add, r=[c_.b_T0, b_id], w=[c_.b_TT]); yield
                Pm = [(c_.P0, c_.b_P0), (c_.P1, c_.b_P1)]; PT = [(c_.T0, c_.b_T0), (c_.T1, c_.b_T1)]
                cur = 0
                for lv in range(5):
                    Pc, b_Pc = Pm[cur]; Tc, b_Tc = PT[cur]; Pn, b_Pn = Pm[1 - cur]; Tn, b_Tn = PT[1 - cur]
                    k.o("pe", "matmul", X1[0:64, 64:128], Tc[:], Pc[:], r=[b_Tc, b_Pc], w=[bX1], **MM)
                    if lv < 4:
                        k.o("pe", "matmul", X1[0:64, 128:192], Pc[:], Tc[:], r=[b_Tc, b_Pc], w=[bX1], **MM)
                    yield
                    k.o("act", "copy", Pn[:], X1[0:64, 64:128], r=[bX1], w=[b_Pn])
                    if lv < 4:
                        k.o("dve", "tensor_copy", Tn[:], X1[0:64, 128:192], r=[bX1], w=[b_Tn])
                    yield
                    k.o("pe", "matmul", X1[0:64, 192:256], Pn[:], c_.TT[:], r=[b_Pn, c_.b_TT], w=[bX1], **MM); yield
                    k.o("dve", "tensor_tensor", c_.TT[:], c_.TT[:], X1[0:64, 192:256], op=ALU.add, r=[c_.b_TT, bX1], w=[c_.b_TT]); yield
                    cur = 1 - cur
                k.o("dve", "tensor_scalar", c_.vb[:], c_.vcs[:, c, :], bcol, None, op0=ALU.mult, r=[c_.b_vcs, c_.b_bts], w=[c_.b_vb])
                k.o("dve", "tensor_scalar", c_.kb[:], c_.kcs[:, c, :], c_.be[:, 0:1], None, op0=ALU.mult, r=[c_.b_kcs, c_.b_be], w=[c_.b_kb])
                k.o("act", "activation", c_.kd[:], c_.kcs[:, c, :], AF.Identity, scale=c_.dec[:, 0:1], r=[c_.b_kcs, c_.b_dec], w=[c_.b_kd]); yield
                k.o("pe", "matmul", X1[0:64, 0:128], c_.TT[:], c_.vb[:], r=[c_.b_TT, c_.b_vb], w=[bX1], **MM)
                k.o("pe", "matmul", X1[0:128, 128:192], c_.kb[:], c_.TT[:], r=[c_.b_TT, c_.b_kb], w=[bX1], **MM); yield
                k.o("act", "copy", c_.u_s[:], X1[0:64, 0:128], r=[bX1], w=[c_.b_u_s])
                k.o("dve", "tensor_copy", c_.wT[:], X1[0:128, 128:192], r=[bX1], w=[c_.b_wT]); yield
                k.o("pe", "matmul", X1[0:64, 192:320], c_.wT[:], c_.St[:], r=[c_.b_wT, c_.b_St], w=[bX1], **MM)
                k.o("pe", "matmul", X1[0:64, 320:448], qTc, c_.St[:], r=[c_.b_qTs, c_.b_St], w=[bX1], **MM); yield
                k.o("dve", "tensor_tensor", c_.vn[:], c_.u_s[:], X1[0:64, 192:320], op=ALU.subtract, r=[c_.b_u_s, bX1], w=[c_.b_vn]); yield
                k.o("pe", "matmul", X1[0:64, 0:128], c_.attT[:], c_.vn[:], r=[c_.b_attT, c_.b_vn], w=[bX1], **MM)
                k.o("pe", "matmul", X0[0:128, 384:512], c_.kd[:], c_.vn[:], r=[c_.b_kd, c_.b_vn], w=[bX0], **MM); yield
                k.o("act", "copy", c_.o2s[:], X1[0:64, 0:128], r=[bX1], w=[c_.b_o2s]); yield
                k.o("dve", "scalar_tensor_tensor", c_.ost[:, c, :], X1[0:64, 320:448], c_.egc[:, 0:1], c_.o2s[:], op0=ALU.mult, op1=ALU.add,
                    r=[bX1, c_.b_egc, c_.b_o2s], w=[c_.b_ost])
                k.o("dve", "scalar_tensor_tensor", c_.St[:], c_.St[:], c_.egl[:, 0:1], X0[0:128, 384:512], op0=ALU.mult, op1=ALU.add,
                    r=[c_.b_St, c_.b_egl, bX0], w=[c_.b_St]); yield
                k.o("act", "activation", c_.ojunk[:], c_.ost[:, c, :], AF.Square, accum_out=c_.ssq[:, 0:1], r=[c_.b_ost], w=[c_.b_ojunk, c_.b_ssq])
                k.o("act", "activation", c_.ssq[:], c_.ssq[:], AF.Sqrt, bias=eps, scale=1.0 / 128, r=[c_.b_ssq], w=[c_.b_ssq]); yield
                k.o("dve", "reciprocal", c_.ssq[:], c_.ssq[:], r=[c_.b_ssq], w=[c_.b_ssq])
                k.o("dve", "scalar_tensor_tensor", c_.ost[:, c, :], c_.ost[:, c, :], c_.ssq[:, 0:1], onwt[:], op0=ALU.mult, op1=ALU.mult,
                    r=[c_.b_ost, c_.b_ssq, b_onw], w=[c_.b_ost])
                k.o("dve", "tensor_tensor", c_.ost[:, c, :], c_.ost[:, c, :], c_.zcs[:, c, :], op=ALU.mult, r=[c_.b_ost, c_.b_zcs], w=[c_.b_ost]); yield
                if c == GC - 1:
                    n0 = n - (GC - 1)
                    k.d("sp", yb[p, n0 * 64:(n + 1) * 64, :].rearrange("(n c) d -> c n d", c=64), c_.ost[:], c_.b_ost, r=[c_.b_ost])

    plists = [[p for p in range(NP) if p % NCHAIN == ci] for ci in range(NCHAIN)]
    gens = [chain_prog(chains[ci], plists[ci]) for ci in range(NCHAIN)]
    live = list(gens)
    while live:
        for gno in list(live):
            try:
                next(gno)
            except StopIteration:
                live.remove(gno)
    return [c_.b_ost for c_ in chains]


def build_mixer(NP, S):
    nc = new_nc()
    k = K(nc)
    NCH = S // 64
    qT = dram_in(nc, "qT", [NP, 128, S]); kT = dram_in(nc, "kT", [NP, 128, S]); v = dram_in(nc, "v", [NP, S, 128])
    qw = dram_in(nc, "qw", [128, 1]); kw = dram_in(nc, "kw", [128, 1]); biasT = dram_in(nc, "biasT", [NP, 5, 128, 128])
    pT = dram_in(nc, "pT", [NP, 3, 128, S]); cw = dram_in(nc, "cw", [NP, 3, 128, 4])
    pb = dram_in(nc, "pb", [NP, 64, NCH]); pg = dram_in(nc, "pg", [NP, 64, NCH])
    alog = dram_in(nc, "alog", [NP, 64, 1]); dtb = dram_in(nc, "dtb", [NP, 64, 1])
    zc = dram_in(nc, "zc", [NP, 64, NCH, 128]); onw = dram_in(nc, "onw", [64, 128])
    ya = dram_out(nc, "ya", [NP, S, 128]); yb = dram_out(nc, "yb", [NP, S, 128])
    oT = nc.dram_tensor("s_oT", [NP, 2, 128, S], F32).ap()
    ktok = nc.dram_tensor("s_ktok", [NP, S, 128], F32).ap(); vtok = nc.dram_tensor("s_vtok", [NP, S, 128], F32).ap()
    sbeta = nc.dram_tensor("s_beta", [NP, 64, NCH], F32).ap(); sg = nc.dram_tensor("s_g", [NP, 64, NCH], F32).ap()
    k.consts()
    k.phase_begin()
    attn_phase(k, NP, S, qT, kT, v, qw, kw, biasT, ya, "a_")
    k.phase_end()
    k.phase_begin()
    dnpre_phase(k, NP, S, pT, cw, pb, pg, alog, dtb, oT, ktok, vtok, sbeta, sg, "d_")
    k.phase_end()
    k.phase_begin()
    dn_phase(k, NP, S, oT[:, 0], oT[:, 1], ktok, vtok, zc, sbeta, sg, onw, yb, [], "n_")
    k.phase_end()
    k.P.emit()
    return nc


HD = 128
NH = 16
AW = NH * HD
OFF_ATT = 0
OFF_DN_QKV = 3 * AW
OFF_DN_GATE = OFF_DN_QKV + 3 * AW
OFF_DN_BETA = OFF_DN_GATE + AW
OFF_DN_DECAY = OFF_DN_BETA + NH
PROJ_COLS = OFF_DN_DECAY + NH
NEXP = 64


def _c(a):
    return np.ascontiguousarray(a, dtype=np.float32)


def _run(nc, maps):
    res = run_bass_kernel_spmd(nc, maps, core_ids=list(range(len(maps))))
    return res.results


def _feat_layout(vec, D):
    return _c(vec.reshape(D // 128, 128).T)


def _make_biasT(rb):
    kk = np.arange(640)[:, None]; qq = np.arange(128)[None, :]
    idx = np.clip((512 + qq) - kk, -256, 256) + 256
    b = rb[idx].astype(np.float32)
    valid = np.where(qq >= 64, kk >= 64, kk < 576)
    return np.where(valid, b, np.float32(-30000.0)).reshape(5, 128, 128)


def run_model(inp, B, S, D, CAP):
    T = B * S
    NC = NCORES
    Tc = T // NC
    f = lambda n: np.asarray(inp[n], dtype=np.float32)
    x = f("x").reshape(T, D)
    w_in = _c(f("w_in")[0])
    cs1 = _feat_layout(f("norm1_w")[0], D)
    nc = build_proj(Tc, D, PROJ_COLS, True, False)
    maps = [{"aT": _c(x[c * Tc:(c + 1) * Tc].T), "W": w_in, "a": _c(x[c * Tc:(c + 1) * Tc]), "cs": cs1} for c in range(NC)]
    p = np.concatenate([r["out"] for r in _run(nc, maps)], 0)
    NP = 2 * B
    NCH = S // 64
    pairs = [[(b, 2 * c + hh) for b in range(B) for hh in range(2)] for c in range(NC)]
    rb = f("rel_bias")[0]
    cwf = f("conv_w")[0]
    nc = build_mixer(NP, S)
    maps = []
    chl = lambda a: a.reshape(NCH, 64).T
    onw = _c(np.tile(f("o_norm_w")[0][None, :], (64, 1)))
    for c in range(NC):
        qT = np.stack([p[b * S:(b + 1) * S, OFF_ATT + h * HD:OFF_ATT + (h + 1) * HD].T for (b, h) in pairs[c]])
        kT = np.stack([p[b * S:(b + 1) * S, AW + h * HD:AW + (h + 1) * HD].T for (b, h) in pairs[c]])
        v = np.stack([p[b * S:(b + 1) * S, 2 * AW + h * HD:2 * AW + (h + 1) * HD] for (b, h) in pairs[c]])
        bT = np.stack([_make_biasT(rb[h]) for (b, h) in pairs[c]])
        pT = np.stack([np.stack([p[b * S:(b + 1) * S, OFF_DN_QKV + cc * AW + h * HD:OFF_DN_QKV + cc * AW + (h + 1) * HD].T for cc in range(3)])
                       for (b, h) in pairs[c]])
        cw = np.stack([np.stack([cwf[:, cc * AW + h * HD:cc * AW + (h + 1) * HD].T for cc in range(3)]) for (b, h) in pairs[c]])
        pb = np.stack([chl(p[b * S:(b + 1) * S, OFF_DN_BETA + h]) for (b, h) in pairs[c]])
        pg = np.stack([chl(p[b * S:(b + 1) * S, OFF_DN_DECAY + h]) for (b, h) in pairs[c]])
        al = np.stack([np.full((64, 1), f("a_log")[0, h], np.float32) for (b, h) in pairs[c]])
        db = np.stack([np.full((64, 1), f("dt_bias")[0, h], np.float32) for (b, h) in pairs[c]])
        zc = np.stack([p[b * S:(b + 1) * S, OFF_DN_GATE + h * HD:OFF_DN_GATE + (h + 1) * HD].reshape(NCH, 64, HD).transpose(1, 0, 2)
                       for (b, h) in pairs[c]])
        maps.append({"qT": _c(qT), "kT": _c(kT), "v": _c(v), "qw": _c(f("q_norm_w")[0].reshape(128, 1)), "kw": _c(f("k_norm_w")[0].reshape(128, 1)),
                     "biasT": _c(bT), "pT": _c(pT), "cw": _c(cw), "pb": _c(pb), "pg": _c(pg), "alog": _c(al), "dtb": _c(db), "zc": _c(zc), "onw": onw})
    res = _run(nc, maps)
    del maps
    y = np.zeros((T, 2 * AW), np.float32)
    for c in range(NC):
        for i, (b, h) in enumerate(pairs[c]):
            y[b * S:(b + 1) * S, h * HD:(h + 1) * HD] = res[c]["ya"][i]
            y[b * S:(b + 1) * S, AW + h * HD:AW + (h + 1) * HD] = res[c]["yb"][i]
    del p, res
    w_out = _c(f("w_out")[0])
    nc = build_proj(Tc, 2 * AW, D, False, True)
    maps = [{"aT": _c(y[c * Tc:(c + 1) * Tc].T), "W": w_out, "res": _c(x[c * Tc:(c + 1) * Tc])} for c in range(NC)]
    x1 = np.concatenate([r["out"] for r in _run(nc, maps)], 0)
    n2 = f("norm2_w")[0]
    Wr = _c(np.concatenate([f("w_group")[0], f("w_router")[0]], 1))
    iot = np.zeros((128, 16), np.float32); iot[:, 0:8] = np.arange(8) * 8; iot[:, 8:16] = np.arange(8)
    nc = build_router(Tc, D)
    maps = [{"a": _c(x1[c * Tc:(c + 1) * Tc]), "aT": _c(x1[c * Tc:(c + 1) * Tc].T), "cs": _feat_layout(n2, D), "csb": _c(np.tile(n2[None, :], (128, 1))),
             "Wr": Wr, "iot": iot} for c in range(NC)]
    res6 = _run(nc, maps)
    h2 = np.concatenate([r["h2"] for r in res6], 0)
    route = np.concatenate([r["route"] for r in res6], 0)
    eid = np.rint(route[:, 0:2]).astype(np.int64)
    flat_e = eid.reshape(-1)
    flat_t = np.repeat(np.arange(T), 2)
    order = np.argsort(flat_e, kind="stable")
    counts = np.bincount(flat_e, minlength=NEXP)
    if counts.max() > CAP:
        raise RuntimeError("expert capacity exceeded: %d > %d" % (counts.max(), CAP))
    starts = np.cumsum(counts) - counts
    rank = np.arange(2 * T) - starts[flat_e[order]]
    slot_sorted = flat_e[order] * CAP + rank
    slot = np.empty(2 * T, np.int64); slot[order] = slot_sorted
    src = np.full(NEXP * CAP, -1, np.int64); src[slot_sorted] = flat_t[order]
    NE = NEXP // NC
    nc = build_experts(D, NE, CAP)
    maps = []
    wgf, wuf, wdf = inp["w_gate"], inp["w_up"], inp["w_down"]
    for c in range(NC):
        s_c = src[c * NE * CAP:(c + 1) * NE * CAP]
        Xc = np.zeros((NE * CAP, D), np.float32)
        m = s_c >= 0
        Xc[m] = h2[s_c[m]]
        maps.append({"XT": _c(Xc.T), "wg": _c(np.asarray(wgf[0, c * NE:(c + 1) * NE])), "wu": _c(np.asarray(wuf[0, c * NE:(c + 1) * NE])),
                     "wd": _c(np.asarray(wdf[0, c * NE:(c + 1) * NE]))})
    Yall = np.concatenate([r["Y"] for r in _run(nc, maps)], 0)
    slot2 = slot.reshape(T, 2)
    nc = build_combine(Tc, D)
    maps = []
    for c in range(NC):
        rows = slice(c * Tc, (c + 1) * Tc)
        maps.append({"x1": _c(x1[rows]), "Y0": _c(Yall[slot2[rows, 0]]), "Y1": _c(Yall[slot2[rows, 1]]), "gt": _c(route[rows, 2:4])})
    out = np.concatenate([r["out"] for r in _run(nc, maps)], 0)
    return out.reshape(B, S, D)


def kernel(**inputs):
    return run_model(inputs, 2, 8192, 4096, 768)
```
